# Optimizing a Trainium2 kernel written in Bass

```python
import math
import jax, jax.numpy as jnp
from jax import lax
import numpy as np

D_MODEL = 4096
BATCH = 2
SEQ = 4096
DEPTH = 1

HEAD_DIM = 128
MIX_WIDTH = D_MODEL
A_HEADS = MIX_WIDTH // (2 * HEAD_DIM)
A_KV_HEADS = A_HEADS // 4
A_GROUP = A_HEADS // A_KV_HEADS
DILATED_BRANCHES = ((128, 1), (512, 4), (2048, 16))
B_HEADS = MIX_WIDTH // (2 * HEAD_DIM)
IDX_HEADS = 16
IDX_DIM = 64
DSA_TOPK_MAX = 256
Q_BLK = 128
PEER_HEADS = 8
PEER_NKEYS = 128
PEER_EXPERTS = PEER_NKEYS * PEER_NKEYS
PEER_QDIM = 256
PEER_HALF = PEER_QDIM // 2
PEER_TOPK = 16
PEER_TOK_BLK = 32
ROPE_THETA = 10000.0
NORM_EPS = 1e-6
NEG = -1e30
ADA_CHUNKS = 6

QA_COLS = A_HEADS * HEAD_DIM
KA_COLS = A_KV_HEADS * HEAD_DIM
VA_COLS = A_KV_HEADS * HEAD_DIM
QB_COLS = B_HEADS * HEAD_DIM
KB_COLS = HEAD_DIM
VB_COLS = HEAD_DIM
QI_COLS = IDX_HEADS * IDX_DIM
KI_COLS = IDX_DIM
WI_COLS = IDX_HEADS
IN_COLS = QA_COLS + KA_COLS + VA_COLS + QB_COLS + KB_COLS + VB_COLS + QI_COLS + KI_COLS + WI_COLS
IN_SPLITS = (QA_COLS,
             QA_COLS + KA_COLS,
             QA_COLS + KA_COLS + VA_COLS,
             QA_COLS + KA_COLS + VA_COLS + QB_COLS,
             QA_COLS + KA_COLS + VA_COLS + QB_COLS + KB_COLS,
             QA_COLS + KA_COLS + VA_COLS + QB_COLS + KB_COLS + VB_COLS,
             QA_COLS + KA_COLS + VA_COLS + QB_COLS + KB_COLS + VB_COLS + QI_COLS,
             QA_COLS + KA_COLS + VA_COLS + QB_COLS + KB_COLS + VB_COLS + QI_COLS + KI_COLS)

kernel_name = "hybrid_dilated_dsa_peer_block"


def rms_norm(x, g):
    xf = x.astype(jnp.float32)
    y = xf * lax.rsqrt(jnp.mean(xf * xf, axis=-1, keepdims=True) + NORM_EPS)
    return (y * g.astype(jnp.float32)).astype(x.dtype)


def rope_tables(positions, dim):
    inv = jnp.power(ROPE_THETA, -jnp.arange(0, dim, 2, dtype=jnp.float32) / dim)
    ang = positions.astype(jnp.float32)[..., None] * inv
    return jnp.cos(ang)[:, :, None, :], jnp.sin(ang)[:, :, None, :]


def apply_rope(t, cos, sin):
    tf = t.astype(jnp.float32)
    half = t.shape[-1] // 2
    t1, t2 = tf[..., :half], tf[..., half:]
    return jnp.concatenate([t1 * cos - t2 * sin, t2 * cos + t1 * sin], axis=-1).astype(t.dtype)


def dilated_branch(q, k, v, window, dil):
    b_, s_, kvh, grp, dh = q.shape
    blk = window // dil
    span = dil * blk
    s_pad = -(-s_ // span) * span
    pad = s_pad - s_
    m = s_pad // dil
    nb = m // blk

    def split(t):
        t = jnp.pad(t, [(0, 0), (0, pad)] + [(0, 0)] * (t.ndim - 2))
        t = t.reshape((b_, m, dil) + t.shape[2:])
        t = jnp.moveaxis(t, 2, 1)
        return t.reshape((b_, dil, nb, blk) + t.shape[3:])

    def with_prev(t):
        prev = jnp.pad(t, [(0, 0), (0, 0), (1, 0)] + [(0, 0)] * (t.ndim - 3))[:, :, :-1]
        return jnp.concatenate([prev, t], axis=3)

    qs = split(q)
    kc = with_prev(split(k))
    vc = with_prev(split(v))
    s = jnp.einsum('brnqkgd,brnjkd->brnkgqj', qs, kc).astype(jnp.float32) * (dh ** -0.5)
    qi = jnp.arange(blk)[:, None]
    kj = jnp.arange(2 * blk)[None, :]
    dist = qi + blk - kj
    band = (dist >= 0) & (dist <= blk)
    first = (jnp.arange(nb) == 0)[:, None, None]
    mask = band[None] & ~(first & (kj < blk)[None])
    s = jnp.where(mask[None, None, :, None, None], s, NEG)
    mx = jnp.max(s, axis=-1, keepdims=True)
    p = jnp.exp(s - mx)
    l = jnp.sum(p, axis=-1, keepdims=True)
    o = jnp.einsum('brnkgqj,brnjkd->brnqkgd', (p / l).astype(v.dtype), vc)
    lse = jnp.moveaxis((mx + jnp.log(l))[..., 0], -1, 3)

    def merge(t):
        t = t.reshape((b_, dil, m) + t.shape[4:])
        t = jnp.moveaxis(t, 1, 2)
        return t.reshape((b_, s_pad) + t.shape[3:])[:, :s_]

    return merge(o), merge(lse)


def dilated_mixture(q, k, v):
    b_, s_ = q.shape[:2]
    qg = q.reshape(b_, s_, A_KV_HEADS, A_GROUP, HEAD_DIM)
    outs, lses = [], []
    for window, dil in DILATED_BRANCHES:
        o, lse = dilated_branch(qg, k, v, window, dil)
        outs.append(o)
        lses.append(lse)
    wts = jax.nn.softmax(jnp.stack(lses, axis=0), axis=0)
    o = jnp.einsum('ibskg,ibskgd->bskgd', wts.astype(q.dtype), jnp.stack(outs, axis=0))
    return o.reshape(b_, s_, A_HEADS * HEAD_DIM)


def dsa_attention(q, k, v, qi, ki, wi):
    b_, s_ = q.shape[:2]
    topk = min(DSA_TOPK_MAX, s_ // 4)
    nb = s_ // Q_BLK

    def blocks(t):
        return jnp.moveaxis(t.reshape((b_, nb, Q_BLK) + t.shape[2:]), 1, 0)

    tpos = jnp.arange(s_, dtype=jnp.int32).reshape(nb, Q_BLK)
    key_pos = jnp.arange(s_, dtype=jnp.int32)
    ki_f = ki.astype(jnp.float32)

    def one_block(args):
        qb, qib, wib, tb = args
        rel = jax.nn.relu(jnp.einsum('bqhd,bsd->bqhs', qib.astype(jnp.float32), ki_f) * (IDX_DIM ** -0.5))
        score = jnp.einsum('bqh,bqhs->bqs', wib.astype(jnp.float32) * (IDX_HEADS ** -0.5), rel)
        causal = key_pos[None, :] <= tb[:, None]
        score = jnp.where(causal[None], score, -jnp.inf)
        _, sel = lax.top_k(score, topk)
        flat = sel.reshape(b_, -1, 1)
        kg = jnp.take_along_axis(k, flat, axis=1).reshape(b_, Q_BLK, topk, HEAD_DIM)
        vg = jnp.take_along_axis(v, flat, axis=1).reshape(b_, Q_BLK, topk, HEAD_DIM)
        s = jnp.einsum('bqhd,bqkd->bqhk', qb, kg).astype(jnp.float32) * (HEAD_DIM ** -0.5)
        valid = sel <= tb[None, :, None]
        s = jnp.where(valid[:, :, None, :], s, NEG)
        p = jax.nn.softmax(s, axis=-1).astype(vg.dtype)
        return jnp.einsum('bqhk,bqkd->bqhd', p, vg)

    out = lax.map(one_block, (blocks(q), blocks(qi), blocks(wi), tpos))
    return jnp.moveaxis(out, 0, 1).reshape(b_, s_, B_HEADS * HEAD_DIM)


def mixing(h, positions, w_in, w_out):
    b_, s_, _ = h.shape
    proj = h @ w_in
    qa, ka, va, qb, kb, vb, qi, ki, wi = jnp.split(proj, IN_SPLITS, axis=-1)
    cos_h, sin_h = rope_tables(positions, HEAD_DIM)
    cos_i, sin_i = rope_tables(positions, IDX_DIM)
    qa = apply_rope(qa.reshape(b_, s_, A_HEADS, HEAD_DIM), cos_h, sin_h)
    ka = apply_rope(ka.reshape(b_, s_, A_KV_HEADS, HEAD_DIM), cos_h, sin_h)
    va = va.reshape(b_, s_, A_KV_HEADS, HEAD_DIM)
    o_a = dilated_mixture(qa, ka, va)
    qb = apply_rope(qb.reshape(b_, s_, B_HEADS, HEAD_DIM), cos_h, sin_h)
    kb = apply_rope(kb[:, :, None, :], cos_h, sin_h)[:, :, 0]
    qi = apply_rope(qi.reshape(b_, s_, IDX_HEADS, IDX_DIM), cos_i, sin_i)
    ki = apply_rope(ki[:, :, None, :], cos_i, sin_i)[:, :, 0]
    o_b = dsa_attention(qb, kb, vb, qi, ki, wi)
    return jnp.concatenate([o_a, o_b], axis=-1) @ w_out


def peer(h, w_q, sub_keys, u_tab, v_tab):
    b_, s_, d = h.shape
    t_ = b_ * s_
    x = h.reshape(t_, d)
    q = (x @ w_q).reshape(t_, PEER_HEADS, 2, PEER_HALF).astype(jnp.float32)
    s = jnp.einsum('thpc,hpnc->thpn', q, sub_keys.astype(jnp.float32))
    v_half, i_half = lax.top_k(s, PEER_TOPK)
    cand = v_half[:, :, 0, :, None] + v_half[:, :, 1, None, :]
    cand_idx = i_half[:, :, 0, :, None] * PEER_NKEYS + i_half[:, :, 1, None, :]
    top_s, pos = lax.top_k(cand.reshape(t_, PEER_HEADS, -1), PEER_TOPK)
    experts = jnp.take_along_axis(cand_idx.reshape(t_, PEER_HEADS, -1), pos, axis=-1)
    gates = jax.nn.softmax(top_s, axis=-1)
    nbk = t_ // PEER_TOK_BLK

    def one_block(args):
        xb, eb, gb = args
        u = jnp.take(u_tab, eb, axis=0)
        a = jnp.einsum('td,thkd->thk', xb, u).astype(jnp.float32)
        act = (jax.nn.gelu(a, approximate=False) * gb).astype(xb.dtype)
        vv = jnp.take(v_tab, eb, axis=0)
        return jnp.einsum('thk,thkd->td', act, vv)

    out = lax.map(one_block, (x.reshape(nbk, PEER_TOK_BLK, d),
                              experts.reshape(nbk, PEER_TOK_BLK, PEER_HEADS, PEER_TOPK),
                              gates.reshape(nbk, PEER_TOK_BLK, PEER_HEADS, PEER_TOPK)))
    return out.reshape(b_, s_, d)


def setup_inputs(seed: int = 0) -> dict:
    key = jax.random.key(seed)
    ks = jax.random.split(key, 16)
    f32 = jnp.float32
    x = jax.random.normal(ks[0], (BATCH, SEQ, D_MODEL), f32)
    c = jax.random.normal(ks[1], (BATCH, D_MODEL), f32)
    offsets = jax.random.randint(ks[2], (BATCH, 1), 0, 1024, dtype=jnp.int32)
    positions = offsets + jnp.arange(SEQ, dtype=jnp.int32)[None, :]
    ln1_g = 1.0 + 0.01 * jax.random.normal(ks[3], (DEPTH, D_MODEL), f32)
    ln2_g = 1.0 + 0.01 * jax.random.normal(ks[4], (DEPTH, D_MODEL), f32)
    w_ada = 0.5 * (D_MODEL ** -0.5) * jax.random.normal(ks[5], (DEPTH, D_MODEL, ADA_CHUNKS * D_MODEL), f32)
    b_ada = 0.02 * jax.random.normal(ks[6], (DEPTH, ADA_CHUNKS * D_MODEL), f32)
    w_in = (D_MODEL ** -0.5) * jax.random.normal(ks[7], (DEPTH, D_MODEL, IN_COLS), f32)
    w_out = (MIX_WIDTH ** -0.5) * jax.random.normal(ks[8], (DEPTH, MIX_WIDTH, D_MODEL), f32)
    peer_wq = (D_MODEL ** -0.5) * jax.random.normal(ks[9], (DEPTH, D_MODEL, PEER_HEADS * PEER_QDIM), f32)
    peer_sub_keys = (PEER_HALF ** -0.5) * jax.random.normal(ks[10], (DEPTH, PEER_HEADS, 2, PEER_NKEYS, PEER_HALF), f32)
    peer_u = (D_MODEL ** -0.5) * jax.random.normal(ks[11], (DEPTH, PEER_EXPERTS, D_MODEL), f32)
    peer_v = (PEER_HEADS ** -0.5) * jax.random.normal(ks[12], (DEPTH, PEER_EXPERTS, D_MODEL), f32)
    lnf_g = 1.0 + 0.01 * jax.random.normal(ks[13], (D_MODEL,), f32)
    return {"x": x, "c": c, "positions": positions, "ln1_g": ln1_g, "ln2_g": ln2_g,
            "w_ada": w_ada, "b_ada": b_ada, "w_in": w_in, "w_out": w_out,
            "peer_wq": peer_wq, "peer_sub_keys": peer_sub_keys, "peer_u": peer_u,
            "peer_v": peer_v, "lnf_g": lnf_g}


def reference(x, c, positions, ln1_g, ln2_g, w_ada, b_ada, w_in, w_out,
              peer_wq, peer_sub_keys, peer_u, peer_v, lnf_g):
    for layer in range(DEPTH):
        mod = jax.nn.silu(c) @ w_ada[layer] + b_ada[layer]
        shift1, scale1, gate1, shift2, scale2, gate2 = [m[:, None, :] for m in jnp.split(mod, ADA_CHUNKS, axis=-1)]
        h = rms_norm(x, ln1_g[layer]) * (1.0 + scale1) + shift1
        x = x + gate1 * mixing(h, positions, w_in[layer], w_out[layer])
        h = rms_norm(x, ln2_g[layer]) * (1.0 + scale2) + shift2
        x = x + gate2 * peer(h, peer_wq[layer], peer_sub_keys[layer], peer_u[layer], peer_v[layer])
    return rms_norm(x, lnf_g)
```

```python
import math
from contextlib import ExitStack

import numpy as np
import concourse.bass as bass
import concourse.mybir as mybir
from concourse.bass_utils import run_bass_kernel_spmd

F32 = mybir.dt.float32
BF16 = mybir.dt.bfloat16
I32 = mybir.dt.int32
U32 = mybir.dt.uint32
AF = mybir.ActivationFunctionType
ALU = mybir.AluOpType
AX = mybir.AxisListType

D = 4096
T = 1024
NT = 8
KC = 32
IN_COLS = 6480
EPS = 1e-6
NEG = -1.0e30
TWO_PI = 2.0 * math.pi


class Res:
    __slots__ = ("name", "w", "rs")

    def __init__(self, name):
        self.name = name
        self.w = None
        self.rs = []


class Prog:
    ENGS = ("pe", "act", "dve", "pool", "sp")

    def __init__(self, nc, es, n_dma_sems=8):
        self.nc = nc
        self.ops = {e: [] for e in self.ENGS}
        self.sem = {}
        self.cnt = {}
        for e in ("pe", "act", "dve", "pool"):
            self.sem[e] = es.enter_context(nc.semaphore("s_" + e))
            self.cnt[e] = 0
        self.dsem = {}
        self.drr = {}
        for q in ("sp", "pool", "act"):
            self.dsem[q] = []
            for k in range(n_dma_sems):
                key = "d_%s%d" % (q, k)
                self.sem[key] = es.enter_context(nc.semaphore(key))
                self.cnt[key] = 0
                self.dsem[q].append(key)
            self.drr[q] = 0
        self.seen = {e: {} for e in self.ENGS}
        self.pend = {e: ([], []) for e in self.ENGS}
        self.nres = 0

    def res(self, name=None):
        self.nres += 1
        return Res(name or ("r%d" % self.nres))

    def _waits(self, e, r, w):
        need = {}

        def add(ref, kind):
            if ref is None:
                return
            key, val, src = ref
            if src == e and (e == "pe" or kind == "war"):
                return
            if need.get(key, 0) < val:
                need[key] = val

        for x in r:
            add(x.w, "raw")
        for x in w:
            add(x.w, "waw")
            for ref in x.rs:
                add(ref, "war")
        out = []
        seen = self.seen[e]
        for key, val in need.items():
            if seen.get(key, 0) < val:
                seen[key] = val
                out.append((key, val))
        return out

    @staticmethod
    def _commit(ref, r, w):
        for x in r:
            x.rs.append(ref)
        for x in w:
            x.w = ref
            x.rs = []

    def op(self, e, fn, r=(), w=(), inc=True):
        r = list(r)
        w = list(w)
        waits = self._waits(e, r, w)
        if not inc:
            self.pend[e][0].extend(r)
            self.pend[e][1].extend(w)
            self.ops[e].append((waits, fn, None))
            return None
        self.cnt[e] += 1
        ref = (e, self.cnt[e], e)
        pr, pw = self.pend[e]
        self._commit(ref, r + pr, w + pw)
        self.pend[e] = ([], [])
        self.ops[e].append((waits, fn, (e, 1)))
        return ref

    def dma(self, q, out, in_, r=(), w=(), **kw):
        r = list(r)
        w = list(w)
        key = self.dsem[q][self.drr[q] % len(self.dsem[q])]
        self.drr[q] += 1
        waits = self._waits(q, r, w)
        prev = self.cnt[key]
        if prev and self.seen[q].get(key, 0) < prev:
            self.seen[q][key] = prev
            waits.append((key, prev))
        self.cnt[key] += 16
        ref = (key, self.cnt[key], "dma_" + q)
        self._commit(ref, r, w)

        def fn(eng, out=out, in_=in_, kw=kw):
            return eng.dma_start(out=out, in_=in_, **kw)

        self.ops[q].append((waits, fn, (key, 16)))
        return ref

    def barrier(self):
        for e in self.ENGS:
            waits = []
            for key, val in self.cnt.items():
                if val and self.seen[e].get(key, 0) < val:
                    self.seen[e][key] = val
                    waits.append((key, val))
            self.ops[e].append((waits, None, None))

    def emit(self):
        nc = self.nc
        sem = self.sem

        def mk(e):
            def body(eng):
                for waits, fn, inc in self.ops[e]:
                    for key, val in waits:
                        eng.wait_ge(sem[key], val)
                    if fn is None:
                        continue
                    ins = fn(eng)
                    if inc is not None:
                        ins.then_inc(sem[inc[0]], inc[1])
            return body

        with nc.Block() as block:
            block.tensor(mk("pe"))
            block.scalar(mk("act"))
            block.vector(mk("dve"))
            block.gpsimd(mk("pool"))
            block.sync(mk("sp"))


class Buf:
    def __init__(self, P, alloc, name, shape, dt, n=1):
        self.t = [alloc("%s%d" % (name, i), shape, dt) for i in range(n)]
        self.r = [P.res("%s%d" % (name, i)) for i in range(n)]
        self.n = n
        self.i = -1

    def next(self):
        self.i = (self.i + 1) % self.n
        return self.t[self.i], self.r[self.i]

    def cur(self):
        return self.t[self.i], self.r[self.i]


def proj_chunks(own):
    ch = []
    if own:
        for c0 in range(0, 2048, 256):
            ch.append((c0, 256, "qa"))
    ch.append((2048, 256, "ka"))
    ch.append((2304, 256, "ka"))
    ch.append((2560, 256, "va"))
    ch.append((2816, 256, "va"))
    if own:
        for c0 in range(3072, 5120, 256):
            ch.append((c0, 256, "qb"))
    ch.append((5120, 256, "kbvb"))
    if own:
        for c0 in range(5376, 6400, 256):
            ch.append((c0, 256, "qi"))
    ch.append((6400, 80, "kiwi"))
    return ch


def build(stage=99, debug=False, only=None, scratch_in=()):
    nc = bass.Bass("TRN2", target_bir_lowering=False)

    def want(name):
        return only is None or name in only

    def din(name, shape, dt=F32):
        return nc.dram_tensor(name, shape, dt, kind="ExternalInput").ap()

    def dscr(name, shape, dt):
        if name in scratch_in:
            return nc.dram_tensor(name, shape, dt, kind="ExternalInput").ap()
        if debug:
            return nc.dram_tensor(name, shape, dt, kind="ExternalOutput").ap()
        return nc.dram_tensor(name, shape, dt).ap()

    xg = din("xg", [4, T, D]) if (want("p1") or want("p2c")) else None
    posT = din("posT", [128, 32], I32)
    c_col = din("c_col", [128, KC])
    ln1_col = din("ln1_col", [128, KC])
    ln2_col = din("ln2_col", [128, KC])
    lnf_row = din("lnf_row", [D])
    b_ada = din("b_ada", [6 * D])
    w_ada = din("w_ada", [D, 6 * D]) if want("p0") else None
    w_in = din("w_in", [D, IN_COLS]) if want("p1") else None
    w_out = din("w_out", [D, D]) if (stage >= 3 and want("p2c")) else None
    wq = din("wq", [D, 2048]) if (stage >= 4 and want("p3")) else None
    subkT = din("subkT", [128, 16, 128])
    if stage >= 4 and want("p4"):
        uT = din("uT", [D, 16384])
        vtab = din("vtab", [16384, D])
    ident_in = din("ident", [128, 128])
    inv_in = din("inv_bc", [128, 96])
    iota_in = din("iota128", [128, 128])
    maskA_in = din("maskA", [128, 15, 128])
    cmaskB_in = din("cmaskB", [128, 4, 128])
    out_d = nc.dram_tensor("out", [T, D], F32, kind="ExternalOutput").ap()

    mod_d = dscr("mod_d", [6 * D], F32)
    qT_d = dscr("qT_d", [32, 128, T], BF16)
    kTa_d = dscr("kTa_d", [4, 128, 4096], BF16)
    va_d = dscr("va_d", [4096, 512], BF16)
    kTb_d = dscr("kTb_d", [128, 4096], BF16)
    vb_d = dscr("vb_d", [4096, 128], BF16)
    kiT_d = dscr("kiT_d", [128, 4096], BF16)
    qiT_d = dscr("qiT_d", [8, 128, T], BF16)
    wi_d = dscr("wi_d", [T, 16], F32)
    oT_d = dscr("oT_d", [32, 128, T], BF16)
    x1_d = dscr("x1_d", [T, D], F32)
    h2T_d = dscr("h2T_d", [128, KC, T], BF16)
    GT_d = dscr("GT_d", [128, 128, T], BF16)
    actg_d = dscr("actg_d", [128, 128, T], BF16)
    y_d = dscr("y_d", [T, D], F32)
    ssq_d = dscr("ssq_d", [2, T], F32)

    with ExitStack() as es:
        P = Prog(nc, es)

        def sb(name, shape, dt):
            return es.enter_context(nc.sbuf_tensor(name, shape, dt))

        identf = sb("identf", [128, 128], F32)
        identb = sb("identb", [128, 128], BF16)
        modcol = sb("modcol", [128, 192], F32)
        A1 = sb("A1", [128, KC], F32)
        A2 = sb("A2", [128, KC], F32)
        ln1c = sb("ln1c", [128, KC], F32)
        ln2c = sb("ln2c", [128, KC], F32)
        ones_f = sb("ones_f", [128, 128], F32)
        r_identf, r_identb, r_modcol, r_A1, r_A2, r_ln, r_ones = (P.res(n) for n in
            ("identf", "identb", "modcol", "A1", "A2", "ln", "ones"))
        P.dma("sp", identf[:], ident_in, w=[r_identf])
        P.dma("sp", ln1c[:], ln1_col, w=[r_ln])
        P.dma("sp", ln2c[:], ln2_col, w=[r_ln])
        P.op("dve", lambda e: e.tensor_copy(out=identb[:], in_=identf[:]), r=[r_identf], w=[r_identb])
        P.op("pool", lambda e: e.memset(ones_f[:], 1.0), w=[r_ones])

        with ExitStack() as ph:
            def sbp(name, shape, dt):
                return ph.enter_context(nc.sbuf_tensor(name, shape, dt))

            def psp(name, shape, dt):
                return ph.enter_context(nc.psum_tensor(name, shape, dt))

            ccol = sbp("ccol", [128, KC], F32)
            scol = sbp("scol", [128, KC], BF16)
            r_c, r_s = P.res("ccol"), P.res("scol")
            P.dma("sp", ccol[:], c_col, w=[r_c])
            P.op("act", lambda e: e.activation(out=scol[:], in_=ccol[:], func=AF.Silu), r=[r_c], w=[r_s])
            wbuf = Buf(P, sbp, "wada", [128, KC, 512], BF16, 2)
            brow = Buf(P, sbp, "brow", [1, 512], F32, 2)
            rowsb = Buf(P, sbp, "rowsb", [1, 512], F32, 2)
            rowps = Buf(P, psp, "rowps", [1, 512], F32, 2)
            colps = Buf(P, psp, "colps", [128, 512], F32, 2)
            w_ada_v = w_ada.rearrange("(kc p) n -> p kc n", p=128) if w_ada is not None else None
            for j in (range(48) if want("p0") else ()):
                wt, wr = wbuf.next()
                P.dma("pool", wt[:], w_ada_v[:, :, j * 512:(j + 1) * 512], w=[wr])
                bt, br = brow.next()
                P.dma("sp", bt[:], b_ada[j * 512:(j + 1) * 512].unsqueeze(0), w=[br])
                pt, pr = rowps.next()
                for kc in range(KC):
                    P.op("pe", lambda e, pt=pt, wt=wt, kc=kc: e.matmul(
                        pt[:], lhsT=scol[:, kc:kc + 1], rhs=wt[:, kc, :],
                        start=(kc == 0), stop=(kc == KC - 1)),
                        r=[r_s, wr], w=[pr], inc=(kc == KC - 1))
                st, sr = rowsb.next()
                P.op("dve", lambda e, st=st, pt=pt, bt=bt: e.tensor_tensor(
                    out=st[:], in0=pt[:], in1=bt[:], op=ALU.add), r=[pr, br], w=[sr])
                P.dma("sp", mod_d[j * 512:(j + 1) * 512].unsqueeze(0), st[:], r=[sr])
                ct, cr = colps.next()
                for k in range(4):
                    P.op("pe", lambda e, ct=ct, st=st, k=k: e.matmul(
                        ct[:, k:k + 1], lhsT=st[0:1, k * 128:(k + 1) * 128], rhs=ones_f[0:1, 0:1],
                        start=True, stop=True), r=[sr, r_ones], w=[cr], inc=(k == 3))
                P.op("act", lambda e, ct=ct, j=j: e.copy(out=modcol[:, j * 4:(j + 1) * 4], in_=ct[:, 0:4]),
                     r=[cr], w=[r_modcol])
            P.op("dve", lambda e: e.scalar_tensor_tensor(
                out=A1[:], in0=modcol[:, 32:64], scalar=1.0, in1=ln1c[:], op0=ALU.add, op1=ALU.mult),
                r=[r_modcol, r_ln], w=[r_A1])
            P.op("dve", lambda e: e.scalar_tensor_tensor(
                out=A2[:], in0=modcol[:, 128:160], scalar=1.0, in1=ln2c[:], op0=ALU.add, op1=ALU.mult),
                r=[r_modcol, r_ln], w=[r_A2])
            P.barrier()
        B1 = modcol[:, 0:32]
        B2 = modcol[:, 96:128]

        if stage >= 1 and want("p1"):
            phase1(nc, P, locals())
        if stage >= 2 and want("p2a"):
            phase2a(nc, P, locals())
        if stage >= 2 and want("p2b"):
            phase2b(nc, P, locals())
        if stage >= 3 and want("p2c"):
            phase2c(nc, P, locals())
        if stage >= 4 and want("p3"):
            phase3(nc, P, locals())
        if stage >= 4 and want("p4"):
            phase4(nc, P, locals())

        P.barrier()
        P.emit()
    return nc


def phase1(nc, P, L):
    xg, posT, w_in = L["xg"], L["posT"], L["w_in"]
    identb, A1, B1 = L["identb"], L["A1"], L["B1"]
    r_identb, r_A1, r_modcol = L["r_identb"], L["r_A1"], L["r_modcol"]
    qT_d, kTa_d, va_d, kTb_d, vb_d, kiT_d, qiT_d, wi_d = (L[k] for k in
        ("qT_d", "kTa_d", "va_d", "kTb_d", "vb_d", "kiT_d", "qiT_d", "wi_d"))
    inv_in = L["inv_in"]
    with ExitStack() as ph:
        def sbp(name, shape, dt):
            return ph.enter_context(nc.sbuf_tensor(name, shape, dt))

        def psp(name, shape, dt):
            return ph.enter_context(nc.psum_tensor(name, shape, dt))

        hT = sbp("hT", [128, KC, T], BF16)
        r_hT = [P.res("hT%d" % s) for s in range(2)]
        xf = Buf(P, sbp, "xf", [128, D], F32, 2)
        xs4 = sbp("xs4", [128, 4, D], BF16)
        r_xs = [P.res("xs%d" % j) for j in range(4)]
        stat = sbp("stat", [128, 8], F32)
        r_stat = P.res("stat")
        wch = Buf(P, sbp, "wch", [128, KC, 256], BF16, 2)
        invb = sbp("invb", [128, 96], F32)
        posi = sbp("posi", [128, 32], I32)
        posf = sbp("posf", [128, 32], F32)
        cosT = sbp("cosT", [128, NT, 96], F32)
        sinT = sbp("sinT", [128, NT, 96], F32)
        nsinT = sbp("nsinT", [128, NT, 96], F32)
        r_tab = [P.res("tab%d" % i) for i in range(NT)]
        tA = sbp("tA", [128, 96], F32)
        tB = sbp("tB", [128, 96], F32)
        tC = sbp("tC", [128, 96], F32)
        tK = sbp("tK", [128, 96], I32)
        r_t = P.res("ttmp")
        r_inv, r_pos = P.res("inv"), P.res("pos")
        tmp1 = Buf(P, sbp, "tmp1", [128, 256], F32, 2)
        tmp2 = Buf(P, sbp, "tmp2", [128, 256], F32, 2)
        obf = Buf(P, sbp, "obf", [128, 256], BF16, 2)
        stT = Buf(P, sbp, "stT", [128, 2, T], BF16, 2)
        stV = Buf(P, sbp, "stV", [128, NT, 256], BF16, 2)
        wist = sbp("wist", [128, NT, 16], F32)
        r_wist = P.res("wist")
        pT = Buf(P, psp, "pT", [128, 512], BF16, 2)
        pp = Buf(P, psp, "pp", [128, 256], F32, 2)
        pq = Buf(P, psp, "pq", [128, 256], BF16, 2)

        P.dma("sp", invb[:], inv_in, w=[r_inv])
        P.dma("sp", posi[:], posT, w=[r_pos])
        P.op("pool", lambda e: e.tensor_copy(out=posf[:], in_=posi[:]), r=[r_pos], w=[r_pos])

        def rope_tables(gt):
            rt = r_tab[gt % NT]
            P.op("pool", lambda e: e.tensor_scalar(out=tA[:], in0=invb[:], scalar1=posf[:, gt:gt + 1],
                                                   scalar2=None, op0=ALU.mult), r=[r_inv, r_pos], w=[r_t])
            P.op("pool", lambda e: e.tensor_scalar(out=tK[:], in0=tA[:], scalar1=1.0 / TWO_PI,
                                                   scalar2=None, op0=ALU.mult), r=[r_t], w=[r_t])
            P.op("pool", lambda e: e.tensor_copy(out=tB[:], in_=tK[:]), r=[r_t], w=[r_t])
            P.op("pool", lambda e: e.tensor_scalar(out=tB[:], in0=tB[:], scalar1=-TWO_PI,
                                                   scalar2=None, op0=ALU.mult), r=[r_t], w=[r_t])
            P.op("pool", lambda e: e.tensor_tensor(out=tA[:], in0=tA[:], in1=tB[:], op=ALU.add), r=[r_t], w=[r_t])

            def wrap(dst, shift):
                P.op("pool", lambda e: e.tensor_scalar(out=dst[:], in0=tA[:], scalar1=shift, scalar2=None,
                                                       op0=ALU.add), r=[r_t], w=[r_t])
                P.op("pool", lambda e: e.tensor_scalar(out=tB[:], in0=dst[:], scalar1=math.pi, scalar2=-TWO_PI,
                                                       op0=ALU.is_gt, op1=ALU.mult), r=[r_t], w=[r_t])
                P.op("pool", lambda e: e.tensor_tensor(out=dst[:], in0=dst[:], in1=tB[:], op=ALU.add), r=[r_t], w=[r_t])
                P.op("pool", lambda e: e.tensor_scalar(out=tB[:], in0=dst[:], scalar1=-math.pi, scalar2=TWO_PI,
                                                       op0=ALU.is_lt, op1=ALU.mult), r=[r_t], w=[r_t])
                P.op("pool", lambda e: e.tensor_tensor(out=dst[:], in0=dst[:], in1=tB[:], op=ALU.add), r=[r_t], w=[r_t])

            wrap(tC, 0.0)
            P.op("act", lambda e: e.activation(out=sinT[:, gt % NT, :], in_=tC[:], func=AF.Sin), r=[r_t], w=[rt])
            P.op("act", lambda e: e.activation(out=nsinT[:, gt % NT, :], in_=tC[:], func=AF.Sin, scale=-1.0), r=[r_t], w=[rt])
            wrap(tC, 0.5 * math.pi)
            P.op("act", lambda e: e.activation(out=cosT[:, gt % NT, :], in_=tC[:], func=AF.Sin), r=[r_t], w=[rt])

        def rope(ppt, ppr, gt, H, half, t0, ocols, cols):
            rt = r_tab[gt % NT]
            gt = gt % NT
            n = H * 2 * half
            t1, r1 = tmp1.next()
            t2, r2 = tmp2.next()
            ot, orr = obf.cur()
            src = ppt[:, cols[0]:cols[0] + n].rearrange("p (h two f) -> p h two f", h=H, two=2)
            cosb = cosT[:, gt, t0:t0 + half].unsqueeze(1).unsqueeze(1).to_broadcast([128, H, 2, half])
            sinb = sinT[:, gt, t0:t0 + half].unsqueeze(1).to_broadcast([128, H, half])
            nsinb = nsinT[:, gt, t0:t0 + half].unsqueeze(1).to_broadcast([128, H, half])
            t1v = t1[:, 0:n].rearrange("p (h two f) -> p h two f", h=H, two=2)
            t2v = t2[:, 0:n].rearrange("p (h two f) -> p h two f", h=H, two=2)
            P.op("dve", lambda e: e.tensor_tensor(out=t1v, in0=src, in1=cosb, op=ALU.mult), r=[ppr, rt], w=[r1])
            P.op("dve", lambda e: e.tensor_tensor(out=t2v[:, :, 0, :], in0=src[:, :, 1, :], in1=nsinb, op=ALU.mult),
                 r=[ppr, rt], w=[r2])
            P.op("dve", lambda e: e.tensor_tensor(out=t2v[:, :, 1, :], in0=src[:, :, 0, :], in1=sinb, op=ALU.mult),
                 r=[ppr, rt], w=[r2])
            P.op("dve", lambda e: e.tensor_tensor(out=ot[:, ocols:ocols + n], in0=t1[:, 0:n], in1=t2[:, 0:n], op=ALU.add),
                 r=[r1, r2], w=[orr])

        w_in_v = w_in.rearrange("(kc p) n -> p kc n", p=128)
        evac_i = [0]
        def norm(g, s):
            for j in range(4):
                tt = s * 4 + j
                gt = g * 8 + tt
                xt, xr = xf.next()
                P.dma("sp", xt[:], xg[g, tt * 128:(tt + 1) * 128, :], w=[xr])
                P.op("act", lambda e, xt=xt, j=j: e.activation(out=xs4[:, j, :], in_=xt[:], func=AF.Square,
                                                            accum_out=stat[:, j:j + 1]),
                     r=[xr], w=[r_xs[j], r_stat])
                P.op("act", lambda e, j=j: e.activation(out=stat[:, j:j + 1], in_=stat[:, j:j + 1], func=AF.Sqrt,
                                                     scale=1.0 / D, bias=EPS),
                     r=[r_stat], w=[r_stat])
                P.op("dve", lambda e, j=j: e.reciprocal(out=stat[:, 4 + j:5 + j], in_=stat[:, j:j + 1]),
                     r=[r_stat], w=[r_stat])
                P.op("dve", lambda e, xt=xt, j=j: e.tensor_scalar(out=xs4[:, j, :], in0=xt[:], scalar1=stat[:, 4 + j:5 + j],
                                                               scalar2=None, op0=ALU.mult),
                     r=[xr, r_stat], w=[r_xs[j]])
                rope_tables(gt)

        def trans(g, s):
            for kc in range(KC):
                pt, pr = pT.next()
                for j in range(4):
                    P.op("pe", lambda e, pt=pt, j=j, kc=kc: e.transpose(
                        out=pt[:, j * 128:(j + 1) * 128], in_=xs4[:, j, kc * 128:(kc + 1) * 128], identity=identb[:]),
                        r=[r_xs[j], r_identb], w=[pr], inc=(j == 3))
                dst = hT[:, kc, s * 512:(s + 1) * 512]
                if evac_i[0] % 2 == 0:
                    P.op("dve", lambda e, pt=pt, dst=dst, kc=kc: e.tensor_scalar(
                        out=dst, in0=pt[:], scalar1=A1[:, kc:kc + 1], scalar2=B1[:, kc:kc + 1],
                        op0=ALU.mult, op1=ALU.add), r=[pr, r_A1, r_modcol], w=[r_hT[s]])
                else:
                    P.op("act", lambda e, pt=pt, dst=dst, kc=kc: e.activation(
                        out=dst, in_=pt[:], func=AF.Identity, scale=A1[:, kc:kc + 1], bias=B1[:, kc:kc + 1]),
                        r=[pr, r_A1, r_modcol], w=[r_hT[s]])
                evac_i[0] += 1

        def mm(g, s):
            own = (g == 0)
            for (c0, wc, kind) in proj_chunks(own):
                wt, wr = wch.next()
                P.dma("pool", wt[:, :, 0:wc], w_in_v[:, :, c0:c0 + wc], w=[wr])
                if kind in ("qa", "qb", "ka", "qi"):
                    sT, sTr = stT.next()
                if kind in ("va",):
                    sV, sVr = stV.next()
                if kind == "kbvb":
                    sT, sTr = stT.next()
                    sV, sVr = stV.next()
                if kind == "kiwi":
                    sT, sTr = stT.next()
                post = [None]
                for tt in range(s * 4, s * 4 + 4):
                    gt = g * 8 + tt
                    ppt, ppr = pp.next()
                    for kc in range(KC):
                        P.op("pe", lambda e, ppt=ppt, wt=wt, kc=kc, tt=tt, wc=wc: e.matmul(
                            ppt[:, 0:wc], lhsT=hT[:, kc, tt * 128:(tt + 1) * 128], rhs=wt[:, kc, 0:wc],
                            start=(kc == 0), stop=(kc == KC - 1)),
                            r=[r_hT[tt // 4], wr], w=[ppr], inc=(kc == KC - 1))
                    if post[0] is not None:
                        post[0]()
                        post[0] = None
                    if kind in ("qa", "qb", "ka"):
                        obf.next()
                        rope(ppt, ppr, gt, 2, 64, 0, 0, (0,))
                        ot, orr = obf.cur()

                        def pst(ot=ot, orr=orr, sT=sT, sTr=sTr, tt=tt):
                            qt, qr = pq.next()
                            for h in range(2):
                                P.op("pe", lambda e, qt=qt, ot=ot, h=h: e.transpose(
                                    out=qt[:, h * 128:(h + 1) * 128], in_=ot[:, h * 128:(h + 1) * 128], identity=identb[:]),
                                    r=[orr, r_identb], w=[qr], inc=(h == 1))
                            P.op("act", lambda e, qt=qt, sT=sT, tt=tt: e.copy(
                                out=sT[:, :, tt * 128:(tt + 1) * 128], in_=qt[:].rearrange("p (h t) -> p h t", h=2)),
                                r=[qr], w=[sTr])
                        post[0] = pst
                    elif kind == "va":
                        P.op("act", lambda e, ppt=ppt, sV=sV, tt=tt: e.copy(out=sV[:, tt, :], in_=ppt[:]),
                             r=[ppr], w=[sVr])
                    elif kind == "kbvb":
                        obf.next()
                        rope(ppt, ppr, gt, 1, 64, 0, 0, (0,))
                        ot, orr = obf.cur()
                        qt, qr = pq.next()
                        P.op("pe", lambda e, qt=qt, ot=ot: e.transpose(
                            out=qt[:, 0:128], in_=ot[:, 0:128], identity=identb[:]), r=[orr, r_identb], w=[qr])
                        P.op("act", lambda e, qt=qt, sT=sT, tt=tt: e.copy(
                            out=sT[:, 0, tt * 128:(tt + 1) * 128], in_=qt[:, 0:128]), r=[qr], w=[sTr])
                        P.op("act", lambda e, ppt=ppt, sV=sV, tt=tt: e.copy(out=sV[:, tt, 0:128], in_=ppt[:, 128:256]),
                             r=[ppr], w=[sVr])
                    elif kind == "qi":
                        obf.next()
                        rope(ppt, ppr, gt, 4, 32, 64, 0, (0,))
                        ot, orr = obf.cur()

                        def pst(ot=ot, orr=orr, sT=sT, sTr=sTr, tt=tt):
                            qt, qr = pq.next()
                            for h in range(2):
                                P.op("pe", lambda e, qt=qt, ot=ot, h=h: e.transpose(
                                    out=qt[:, h * 128:(h + 1) * 128], in_=ot[:, h * 128:(h + 1) * 128], identity=identb[:]),
                                    r=[orr, r_identb], w=[qr], inc=(h == 1))
                            P.op("act", lambda e, qt=qt, sT=sT, tt=tt: e.copy(
                                out=sT[:, :, tt * 128:(tt + 1) * 128], in_=qt[:].rearrange("p (h t) -> p h t", h=2)),
                                r=[qr], w=[sTr])
                        post[0] = pst
                    elif kind == "kiwi":
                        obf.next()
                        rope(ppt, ppr, gt, 1, 32, 64, 0, (0,))
                        rope(ppt, ppr, gt, 1, 32, 64, 64, (0,))
                        ot, orr = obf.cur()
                        qt, qr = pq.next()
                        P.op("pe", lambda e, qt=qt, ot=ot: e.transpose(
                            out=qt[:, 0:128], in_=ot[:, 0:128], identity=identb[:]), r=[orr, r_identb], w=[qr])
                        P.op("act", lambda e, qt=qt, sT=sT, tt=tt: e.copy(
                            out=sT[:, 0, tt * 128:(tt + 1) * 128], in_=qt[:, 0:128]), r=[qr], w=[sTr])
                        if own:
                            P.op("act", lambda e, ppt=ppt, tt=tt: e.copy(out=wist[:, tt, :], in_=ppt[:, 64:80]),
                                 r=[ppr], w=[r_wist])
                if post[0] is not None:
                    post[0]()
                    post[0] = None
                if kind in ("qa", "qb"):
                    h0 = (c0 // 128) if kind == "qa" else 16 + (c0 - 3072) // 128
                    P.dma("sp", qT_d[h0:h0 + 2, :, s * 512:(s + 1) * 512].rearrange("h p t -> p h t"), sT[:, :, s * 512:(s + 1) * 512], r=[sTr])
                elif kind == "ka":
                    h0 = (c0 - 2048) // 128
                    for h in range(2):
                        dst = kTa_d[h0 + h].rearrange("p (tt gg i) -> p tt gg i", tt=NT, gg=4)[:, s * 4:(s + 1) * 4, g, :]
                        P.dma("sp", dst, sT[:, h, :].rearrange("p (tt i) -> p tt i", tt=NT)[:, s * 4:(s + 1) * 4, :], r=[sTr])
                elif kind == "va":
                    cc = c0 - 2560
                    dst = va_d.rearrange("(tt gg i) c -> i tt gg c", tt=NT, gg=4)[:, s * 4:(s + 1) * 4, g, cc:cc + 256]
                    P.dma("sp", dst, sV[:, s * 4:(s + 1) * 4, :], r=[sVr])
                elif kind == "kbvb":
                    dst = kTb_d.rearrange("p (tt gg i) -> p tt gg i", tt=NT, gg=4)[:, s * 4:(s + 1) * 4, g, :]
                    P.dma("sp", dst, sT[:, 0, :].rearrange("p (tt i) -> p tt i", tt=NT)[:, s * 4:(s + 1) * 4, :], r=[sTr])
                    dst = vb_d.rearrange("(tt gg i) c -> i tt gg c", tt=NT, gg=4)[:, s * 4:(s + 1) * 4, g, :]
                    P.dma("sp", dst, sV[:, s * 4:(s + 1) * 4, 0:128], r=[sVr])
                elif kind == "qi":
                    h0 = (c0 - 5376) // 128
                    P.dma("sp", qiT_d[h0:h0 + 2, :, s * 512:(s + 1) * 512].rearrange("h p t -> p h t"), sT[:, :, s * 512:(s + 1) * 512], r=[sTr])
                elif kind == "kiwi":
                    dst = kiT_d.rearrange("p (tt gg i) -> p tt gg i", tt=NT, gg=4)[:, s * 4:(s + 1) * 4, g, :]
                    P.dma("sp", dst, sT[:, 0, :].rearrange("p (tt i) -> p tt i", tt=NT)[:, s * 4:(s + 1) * 4, :], r=[sTr])
                    if own:
                        P.dma("sp", wi_d.rearrange("(tt i) c -> i tt c", tt=NT)[:, s * 4:(s + 1) * 4, :], wist[:, s * 4:(s + 1) * 4, :], r=[r_wist])

        units = [(g, s) for g in (1, 2, 3, 0) for s in range(2)]
        norm(*units[0])
        for k_, (g, s) in enumerate(units):
            trans(g, s)
            if k_ + 1 < len(units):
                norm(*units[k_ + 1])
            mm(g, s)
        P.barrier()


def rope_inv():
    inv128 = np.power(np.float32(10000.0), -np.arange(0, 128, 2, dtype=np.float32) / np.float32(128)).astype(np.float32)
    inv64 = np.power(np.float32(10000.0), -np.arange(0, 64, 2, dtype=np.float32) / np.float32(64)).astype(np.float32)
    return np.concatenate([inv128, inv64]).astype(np.float32)


def col_layout(v):
    return np.ascontiguousarray(np.asarray(v, dtype=np.float32).reshape(KC, 128).T)


def prep_shared(inputs, stage):
    sh = {
        "ln1_col": col_layout(inputs["ln1_g"][0]),
        "ln2_col": col_layout(inputs["ln2_g"][0]),
        "lnf_row": np.ascontiguousarray(inputs["lnf_g"], dtype=np.float32),
        "b_ada": np.ascontiguousarray(inputs["b_ada"][0], dtype=np.float32),
        "w_ada": np.ascontiguousarray(inputs["w_ada"][0], dtype=np.float32),
        "w_in": np.ascontiguousarray(inputs["w_in"][0], dtype=np.float32),
        "w_out": np.ascontiguousarray(inputs["w_out"][0], dtype=np.float32),
        "wq": np.ascontiguousarray(inputs["peer_wq"][0], dtype=np.float32),
        "subkT": np.ascontiguousarray(np.asarray(inputs["peer_sub_keys"][0], dtype=np.float32)
                                      .reshape(16, 128, 128).transpose(2, 0, 1)),
        "ident": np.eye(128, dtype=np.float32),
        "inv_bc": np.ascontiguousarray(np.broadcast_to(rope_inv()[None, :], (128, 96))),
        "iota128": np.ascontiguousarray(np.broadcast_to(np.arange(128, dtype=np.float32)[None, :], (128, 128))),
    }
    if stage >= 4:
        sh["uT"] = np.ascontiguousarray(np.asarray(inputs["peer_u"][0], dtype=np.float32).T)
        sh["vtab"] = np.ascontiguousarray(inputs["peer_v"][0], dtype=np.float32)
    return sh


def prep_core(inputs, shared, b, q):
    x = np.asarray(inputs["x"], dtype=np.float32)
    pos = np.asarray(inputs["positions"], dtype=np.int32)
    xg = np.stack([x[b, ((q + g) % 4)::4, :] for g in range(4)], axis=0)
    pg = np.stack([pos[b, ((q + g) % 4)::4] for g in range(4)], axis=0)
    posT = np.ascontiguousarray(pg.reshape(4, NT, 128).transpose(2, 0, 1).reshape(128, 32))
    m = dict(shared)
    m["xg"] = np.ascontiguousarray(xg)
    m["posT"] = posT.astype(np.int32)
    m["c_col"] = col_layout(np.asarray(inputs["c"], dtype=np.float32)[b])
    mA, cB = make_masks(q)
    m["maskA"] = mA
    m["cmaskB"] = cB
    return m


def sm_front(P, W, S_sb, S_r, Pt, P_r, st):
    scale = 128 ** -0.5
    stt, r_st = st
    P.op("dve", lambda e: e.tensor_reduce(out=stt[:, 0:1], in_=S_sb[:, 0:W], axis=AX.X, op=ALU.max),
         r=[S_r], w=[r_st])
    P.op("dve", lambda e: e.tensor_scalar(out=stt[:, 1:2], in0=stt[:, 0:1], scalar1=-scale, scalar2=None, op0=ALU.mult),
         r=[r_st], w=[r_st])
    P.op("act", lambda e: e.activation(out=Pt[:, 0:W], in_=S_sb[:, 0:W], func=AF.Exp, scale=scale,
                                       bias=stt[:, 1:2], accum_out=stt[:, 2:3]),
         r=[S_r, r_st], w=[P_r, r_st])


def sm_back(P, W, Pt, P_r, PTt, PT_r, PTps, Ops, vtile, oTst, oT_r, hslot, st, r_identb, identb):
    stt, r_st = st
    P.op("dve", lambda e: e.reciprocal(out=stt[:, 3:4], in_=stt[:, 2:3]), r=[r_st], w=[r_st])
    P.op("dve", lambda e: e.tensor_scalar(out=Pt[:, 0:W], in0=Pt[:, 0:W], scalar1=stt[:, 3:4], scalar2=None, op0=ALU.mult),
         r=[P_r, r_st], w=[P_r])
    ntile = W // 128
    for c0 in range(0, ntile, 8):
        c1 = min(ntile, c0 + 8)
        pt, pr = PTps.next()
        for k in range(c0, c1):
            P.op("pe", lambda e, pt=pt, k=k, c0=c0: e.transpose(
                out=pt[:, (k - c0) * 128:(k - c0 + 1) * 128], in_=Pt[:, k * 128:(k + 1) * 128], identity=identb[:]),
                r=[P_r, r_identb], w=[pr], inc=(k == c1 - 1))
        P.op("act", lambda e, pt=pt, c0=c0, c1=c1: e.copy(out=PTt[:, c0 * 128:c1 * 128], in_=pt[:, 0:(c1 - c0) * 128]),
             r=[pr], w=[PT_r])
    ot, orr = Ops.next()
    for k in range(ntile):
        vt, vr = vtile(k)
        P.op("pe", lambda e, ot=ot, vt=vt, k=k: e.matmul(
            ot[:, 0:128], lhsT=vt, rhs=PTt[:, k * 128:(k + 1) * 128], start=(k == 0), stop=(k == ntile - 1)),
            r=[PT_r, vr], w=[orr], inc=(k == ntile - 1))
    P.op("act", lambda e, ot=ot: e.copy(out=oTst[:, hslot, :], in_=ot[:, 0:128]), r=[orr], w=[oT_r])


def phase2a(nc, P, L):
    identb, r_identb = L["identb"], L["r_identb"]
    qT_d, kTa_d, va_d, oT_d, maskA_in = (L[k] for k in ("qT_d", "kTa_d", "va_d", "oT_d", "maskA_in"))
    with ExitStack() as ph:
        def sbp(name, shape, dt):
            return ph.enter_context(nc.sbuf_tensor(name, shape, dt))

        def psp(name, shape, dt):
            return ph.enter_context(nc.psum_tensor(name, shape, dt))

        KT = sbp("KTa", [128, 4, 4096], BF16)
        VA = sbp("VAr", [128, 32, 512], BF16)
        mA = sbp("mA", [128, 15, 128], F32)
        r_KT, r_VA, r_mA = P.res("KTa"), P.res("VAr"), P.res("mA")
        for h in range(4):
            P.dma("sp", KT[:, h, :], kTa_d[h], w=[r_KT])
        P.dma("sp", VA[:], va_d.rearrange("(tl p) c -> p tl c", p=128), w=[r_VA])
        P.dma("sp", mA[:], maskA_in, w=[r_mA])
        qblk = Buf(P, sbp, "qblkA", [128, 16, 128], BF16, 2)
        Ssb = Buf(P, sbp, "SsbA", [128, 1920], F32, 2)
        Pb = Buf(P, sbp, "PbA", [128, 1920], BF16, 2)
        PTb = Buf(P, sbp, "PTbA", [128, 1920], BF16, 2)
        oTst = Buf(P, sbp, "oTstA", [128, 16, 128], BF16, 2)
        stb = Buf(P, sbp, "stA", [128, 4], F32, 2)
        Sps = Buf(P, psp, "SpsA", [128, 512], F32, 4)
        PTps = Buf(P, psp, "PTpsA", [128, 1024], BF16, 2)
        Ops = Buf(P, psp, "OpsA", [128, 512], F32, 2)
        pending = [None]
        for n in range(NT):
            qt, qr = qblk.next()
            P.dma("sp", qt[:], qT_d[0:16, :, n * 128:(n + 1) * 128].rearrange("h p t -> p h t"), w=[qr])
            ost, osr = oTst.next()
            slots = []
            if n > 0:
                slots.append(((n - 1) * 512, 512, 0))
            slots.append((n * 512, 512, 4))
            if n > 0:
                slots.append(((n - 1) * 512, 128, 8))
            slots.append((n * 512, 128, 9))
            for k in range(4, -1, -1):
                if n - k >= 0:
                    slots.append(((n - k) * 512, 128, 10 + (4 - k)))
            W = sum(sl[1] for sl in slots)
            for kv in range(4):
                for hq in range(4):
                    head = kv * 4 + hq
                    St, Sr = Ssb.next()
                    off = 0
                    bank, bw = None, 512
                    groups = []
                    for (p0, w, m0) in slots:
                        if bw + w > 512:
                            bank = Sps.next()
                            groups.append([bank, []])
                            bw = 0
                        groups[-1][1].append((bw, p0, w, m0))
                        bw += w
                    for (bt, br), items in groups:
                        for i, (bo, p0, w, m0) in enumerate(items):
                            P.op("pe", lambda e, bt=bt, bo=bo, p0=p0, w=w, kv=kv, head=head, qt=qt: e.matmul(
                                bt[:, bo:bo + w], lhsT=qt[:, head, :], rhs=KT[:, kv, p0:p0 + w], start=True, stop=True),
                                r=[qr, r_KT], w=[br], inc=(i == len(items) - 1))
                        runs = []
                        for (bo, p0, w, m0) in items:
                            if runs and runs[-1][2] + runs[-1][1] // 128 == m0:
                                runs[-1][1] += w
                            else:
                                runs.append([bo, w, m0])
                        for (bo, tw, m0) in runs:
                            P.op("dve", lambda e, bt=bt, St=St, off=off, tw=tw, m0=m0, bo=bo: e.tensor_tensor(
                                out=St[:, off:off + tw], in0=bt[:, bo:bo + tw],
                                in1=mA[:, m0:m0 + tw // 128, :].rearrange("p a b -> p (a b)"), op=ALU.add),
                                r=[br, r_mA], w=[Sr])
                            off += tw
                    vt_list = []
                    for (p0, w, m0) in slots:
                        for k in range(w // 128):
                            vt_list.append(p0 // 128 + k)
                    Pt, Pr = Pb.next()
                    PTt, PTr = PTb.next()
                    st = stb.next()
                    sm_front(P, W, St, Sr, Pt, Pr, st)

                    def back(W=W, Pt=Pt, Pr=Pr, PTt=PTt, PTr=PTr, kv=kv, vt_list=vt_list, ost=ost, osr=osr,
                             head=head, st=st, n=n):
                        sm_back(P, W, Pt, Pr, PTt, PTr, PTps, Ops,
                                lambda k: (VA[:, vt_list[k], kv * 128:(kv + 1) * 128], r_VA),
                                ost, osr, head, st, r_identb, identb)
                        if head == 15:
                            P.dma("sp", oT_d[0:16, :, n * 128:(n + 1) * 128].rearrange("h p t -> p h t"), ost[:], r=[osr])

                    if pending[0] is not None:
                        pending[0]()
                    pending[0] = back
        if pending[0] is not None:
            pending[0]()
        P.barrier()


def phase2b(nc, P, L):
    identb, r_identb = L["identb"], L["r_identb"]
    qT_d, kTb_d, vb_d, kiT_d, qiT_d, wi_d, oT_d, cmaskB_in = (L[k] for k in
        ("qT_d", "kTb_d", "vb_d", "kiT_d", "qiT_d", "wi_d", "oT_d", "cmaskB_in"))
    NIT = 26
    with ExitStack() as ph:
        def sbp(name, shape, dt):
            return ph.enter_context(nc.sbuf_tensor(name, shape, dt))

        def psp(name, shape, dt):
            return ph.enter_context(nc.psum_tensor(name, shape, dt))

        KB = sbp("KTb", [128, 4096], BF16)
        VB = sbp("VBr", [128, 32, 128], BF16)
        KI = sbp("KIr", [128, 4096], BF16)
        cm = sbp("cmB", [128, 4, 128], F32)
        r_KB, r_VB, r_KI, r_cm = P.res("KTb"), P.res("VBr"), P.res("KIr"), P.res("cmB")
        P.dma("sp", KB[:], kTb_d, w=[r_KB])
        P.dma("sp", VB[:], vb_d.rearrange("(tl p) c -> p tl c", p=128), w=[r_VB])
        P.dma("sp", KI[:], kiT_d, w=[r_KI])
        P.dma("sp", cm[:], cmaskB_in, w=[r_cm])
        qblk = Buf(P, sbp, "qblkB", [128, 16, 128], BF16, 2)
        qiblk = Buf(P, sbp, "qiblk", [128, 8, 128], BF16, 2)
        wib = Buf(P, sbp, "wib", [128, 16], F32, 2)
        scoreb = Buf(P, sbp, "score", [128, 4096], F32, 2)
        addmb = Buf(P, sbp, "addm", [128, 4096], BF16, 2)
        junkb = sbp("junkb", [128, 4096], BF16)
        r_junkb = P.res("junkb")
        relb = Buf(P, sbp, "relb", [128, 512], F32, 4)
        bstb = Buf(P, sbp, "bst", [128, 8], F32, 2)
        Ssb = Buf(P, sbp, "SsbB", [128, 4096], F32, 2)
        Pb = Buf(P, sbp, "PbB", [128, 4096], BF16, 2)
        PTb = Buf(P, sbp, "PTbB", [128, 4096], BF16, 2)
        oTst = Buf(P, sbp, "oTstB", [128, 16, 128], BF16, 2)
        stb = Buf(P, sbp, "stB", [128, 4], F32, 2)
        Rps = Buf(P, psp, "RpsB", [128, 512], F32, 2)
        Sps = Buf(P, psp, "SpsB", [128, 512], F32, 2)
        PTps = Buf(P, psp, "PTpsB", [128, 1024], BF16, 2)
        Ops = Buf(P, psp, "OpsB", [128, 512], F32, 2)

        def front(n, out):
            Kn = (n + 1) * 512
            nch = n + 1
            qt, qr = qblk.next()
            P.dma("sp", qt[:], qT_d[16:32, :, n * 128:(n + 1) * 128].rearrange("h p t -> p h t"), w=[qr])
            qit, qir = qiblk.next()
            P.dma("sp", qit[:], qiT_d[:, :, n * 128:(n + 1) * 128].rearrange("h p t -> p h t"), w=[qir])
            wt, wr = wib.next()
            P.dma("sp", wt[:], wi_d[n * 128:(n + 1) * 128, :], w=[wr])
            score, r_score = scoreb.next()
            addm, r_addm = addmb.next()
            bst, r_bst = bstb.next()
            out.update(qt=qt, qr=qr, addm=addm, r_addm=r_addm, Kn=Kn, nch=nch)
            yield
            for hp in range(8):
                for half in range(2):
                    hd = hp * 2 + half
                    ps0 = half * 64
                    for kc in range(nch):
                        rt, rr = Rps.next()
                        P.op("pe", lambda e, rt=rt, qit=qit, hp=hp, ps0=ps0, kc=kc: e.matmul(
                            rt[:], lhsT=qit[ps0:ps0 + 64, hp, :], rhs=KI[ps0:ps0 + 64, kc * 512:(kc + 1) * 512],
                            start=True, stop=True), r=[qir, r_KI], w=[rr])
                        lt, lr = relb.next()
                        P.op("act", lambda e, rt=rt, lt=lt: e.activation(out=lt[:], in_=rt[:], func=AF.Relu, scale=1.0 / 32.0),
                             r=[rr], w=[lr])
                        dst = score[:, kc * 512:(kc + 1) * 512]
                        if hd == 0:
                            P.op("act", lambda e, lt=lt, dst=dst, wt=wt, hd=hd: e.activation(
                                out=dst, in_=lt[:], func=AF.Identity, scale=wt[:, hd:hd + 1]),
                                r=[lr, wr], w=[r_score])
                        else:
                            P.op("act", lambda e, lt=lt, wt=wt, hd=hd: e.activation(
                                out=lt[:], in_=lt[:], func=AF.Identity, scale=wt[:, hd:hd + 1]),
                                r=[lr, wr], w=[lr])
                            P.op("pool", lambda e, lt=lt, dst=dst: e.tensor_tensor(out=dst, in0=dst, in1=lt[:], op=ALU.add),
                                 r=[lr, r_score], w=[r_score])
                        yield
            dg = score[:, Kn - 512:Kn]
            P.op("dve", lambda e, dg=dg: e.tensor_tensor(out=dg, in0=dg, in1=cm[:].rearrange("p a b -> p (a b)"), op=ALU.add),
                 r=[r_score, r_cm], w=[r_score])
            P.op("dve", lambda e: e.tensor_reduce(out=bst[:, 0:1], in_=score[:, 0:Kn], axis=AX.X, op=ALU.max),
                 r=[r_score], w=[r_bst])
            P.op("dve", lambda e: e.tensor_scalar(out=bst[:, 1:2], in0=bst[:, 0:1], scalar1=-64.0, scalar2=None, op0=ALU.add),
                 r=[r_bst], w=[r_bst])
            yield
            for it in range(NIT):
                ck = 64.0 / (2.0 ** (it + 1))
                P.op("dve", lambda e, ck=ck: e.tensor_scalar(out=bst[:, 2:3], in0=bst[:, 1:2], scalar1=ck, scalar2=None, op0=ALU.add),
                     r=[r_bst], w=[r_bst])
                P.op("dve", lambda e: e.tensor_scalar(out=junkb[:, 0:Kn], in0=score[:, 0:Kn], scalar1=bst[:, 2:3],
                                                      scalar2=None, op0=ALU.is_ge, op1=ALU.add, accum_out=bst[:, 3:4]),
                     r=[r_score, r_bst], w=[r_junkb, r_bst])
                yield
                P.op("dve", lambda e: e.tensor_scalar(out=bst[:, 4:5], in0=bst[:, 3:4], scalar1=255.5, scalar2=None, op0=ALU.is_gt),
                     r=[r_bst], w=[r_bst])
                P.op("dve", lambda e, ck=ck: e.scalar_tensor_tensor(out=bst[:, 1:2], in0=bst[:, 4:5], scalar=ck, in1=bst[:, 1:2],
                                                                 op0=ALU.mult, op1=ALU.add), r=[r_bst], w=[r_bst])
                yield
            P.op("dve", lambda e: e.tensor_scalar(out=addm[:, 0:Kn], in0=score[:, 0:Kn], scalar1=bst[:, 1:2], scalar2=NEG,
                                                  op0=ALU.is_lt, op1=ALU.mult), r=[r_score, r_bst], w=[r_addm])
            yield

        def nsteps(n):
            return 1 + 16 * (n + 1) + 1 + 2 * NIT + 1

        cur = {}
        pending = [None]
        for _ in front(0, cur):
            pass
        for n in range(NT):
            nxt = {}
            gen = front(n + 1, nxt) if n + 1 < NT else None
            per_head = (nsteps(n + 1) + 15) // 16 if gen is not None else 0
            qt, qr, addm, r_addm, Kn, nch = (cur[k] for k in ("qt", "qr", "addm", "r_addm", "Kn", "nch"))
            ost, osr = oTst.next()
            for h in range(16):
                St, Sr = Ssb.next()
                for kc in range(nch):
                    bt, br = Sps.next()
                    P.op("pe", lambda e, bt=bt, qt=qt, h=h, kc=kc: e.matmul(
                        bt[:], lhsT=qt[:, h, :], rhs=KB[:, kc * 512:(kc + 1) * 512], start=True, stop=True),
                        r=[qr, r_KB], w=[br])
                    P.op("dve", lambda e, bt=bt, St=St, kc=kc, addm=addm: e.tensor_tensor(
                        out=St[:, kc * 512:(kc + 1) * 512], in0=bt[:], in1=addm[:, kc * 512:(kc + 1) * 512], op=ALU.add),
                        r=[br, r_addm], w=[Sr])
                Pt, Pr = Pb.next()
                PTt, PTr = PTb.next()
                st = stb.next()
                sm_front(P, Kn, St, Sr, Pt, Pr, st)

                def back(Kn=Kn, Pt=Pt, Pr=Pr, PTt=PTt, PTr=PTr, ost=ost, osr=osr, h=h, st=st, n=n):
                    sm_back(P, Kn, Pt, Pr, PTt, PTr, PTps, Ops, lambda k: (VB[:, k, :], r_VB),
                            ost, osr, h, st, r_identb, identb)
                    if h == 15:
                        P.dma("sp", oT_d[16:32, :, n * 128:(n + 1) * 128].rearrange("h p t -> p h t"), ost[:], r=[osr])

                if pending[0] is not None:
                    pending[0]()
                pending[0] = back
                if gen is not None:
                    for _ in range(per_head):
                        try:
                            next(gen)
                        except StopIteration:
                            gen = None
                            break
            if gen is not None:
                for _ in gen:
                    pass
            cur = nxt
        if pending[0] is not None:
            pending[0]()
        P.barrier()


def phase2c(nc, P, L):
    xg, w_out, oT_d, x1_d, mod_d = (L[k] for k in ("xg", "w_out", "oT_d", "x1_d", "mod_d"))
    with ExitStack() as ph:
        def sbp(name, shape, dt):
            return ph.enter_context(nc.sbuf_tensor(name, shape, dt))

        def psp(name, shape, dt):
            return ph.enter_context(nc.psum_tensor(name, shape, dt))

        oT = sbp("oTall", [128, 32, T], BF16)
        r_oT = P.res("oTall")
        P.dma("sp", oT[:], oT_d.rearrange("h p t -> p h t"), w=[r_oT])
        g1 = sbp("g1bc", [128, D], F32)
        r_g1 = P.res("g1bc")
        P.dma("sp", g1[:], mod_d[2 * D:3 * D].partition_broadcast(128), w=[r_g1])
        wb = Buf(P, sbp, "woutb", [128, 32, 512], BF16, 2)
        xb = Buf(P, sbp, "xres", [128, 512], F32, 3)
        yb = Buf(P, sbp, "ytmp", [128, 512], F32, 3)
        yps = Buf(P, psp, "yps", [128, 512], F32, 4)
        w_out_v = w_out.rearrange("(h p) n -> p h n", p=128)
        for nc_ in range(8):
            wt, wr = wb.next()
            P.dma("pool", wt[:], w_out_v[:, :, nc_ * 512:(nc_ + 1) * 512], w=[wr])
            for tt in range(NT):
                xt, xr = xb.next()
                P.dma("sp", xt[:], xg[0, tt * 128:(tt + 1) * 128, nc_ * 512:(nc_ + 1) * 512], w=[xr])
                pt, pr = yps.next()
                for h in range(32):
                    P.op("pe", lambda e, pt=pt, wt=wt, h=h, tt=tt: e.matmul(
                        pt[:], lhsT=oT[:, h, tt * 128:(tt + 1) * 128], rhs=wt[:, h, :], start=(h == 0), stop=(h == 31)),
                        r=[r_oT, wr], w=[pr], inc=(h == 31))
                yt, yr = yb.next()
                P.op("dve", lambda e, pt=pt, yt=yt, nc_=nc_: e.tensor_tensor(
                    out=yt[:], in0=pt[:], in1=g1[:, nc_ * 512:(nc_ + 1) * 512], op=ALU.mult), r=[pr, r_g1], w=[yr])
                P.op("pool", lambda e, yt=yt, xt=xt: e.tensor_tensor(out=yt[:], in0=yt[:], in1=xt[:], op=ALU.add),
                     r=[yr, xr], w=[yr])
                P.dma("sp", x1_d[tt * 128:(tt + 1) * 128, nc_ * 512:(nc_ + 1) * 512], yt[:], r=[yr])
        P.barrier()


def make_masks(q):
    p = np.arange(128)[:, None]
    j = np.arange(128)[None, :]
    mA = np.zeros((128, 15, 128), dtype=np.float32)

    def put(idx, valid):
        mA[:, idx, :] = np.where(valid, 0.0, NEG).astype(np.float32)

    for g in range(4):
        dg = q - ((q + g) % 4)
        d1 = 4 * (128 + p - j) + dg
        put(g, (d1 >= 0) & (d1 <= 128))
        d0 = 4 * (p - j) + dg
        put(4 + g, (d0 >= 0) & (d0 <= 128))
    put(8, j >= p)
    put(9, j <= p)
    for k in range(4, -1, -1):
        df = 128 * k + p - j
        put(10 + (4 - k), (df % 4 == 0) & (df >= 0) & (df <= 512))
    cB = np.zeros((128, 4, 128), dtype=np.float32)
    for g in range(4):
        rg = (q + g) % 4
        cB[:, g, :] = np.where((j < p) | ((j == p) & (rg <= q)), 0.0, NEG)
    return mA, cB


def phase3(nc, P, L):
    x1_d, wq, subkT, h2T_d, GT_d, iota_in = (L[k] for k in ("x1_d", "wq", "subkT", "h2T_d", "GT_d", "iota_in"))
    identb, r_identb, identf, r_identf = L["identb"], L["r_identb"], L["identf"], L["r_identf"]
    A2, B2, r_A2, r_modcol = L["A2"], L["B2"], L["r_A2"], L["r_modcol"]
    with ExitStack() as ph3:
        qpT = ph3.enter_context(nc.sbuf_tensor("qpT", [128, 16, T], F32))
        r_qp = [P.res("qpT%d" % i) for i in range(2)]
        with ExitStack() as ph:
            def sbp(name, shape, dt):
                return ph.enter_context(nc.sbuf_tensor(name, shape, dt))

            def psp(name, shape, dt):
                return ph.enter_context(nc.psum_tensor(name, shape, dt))

            hT = sbp("h2T", [128, KC, T], BF16)
            r_hT = [P.res("h2T%d" % i) for i in range(2)]
            xf = Buf(P, sbp, "x1f", [128, D], F32, 1)
            xs4 = sbp("x1s4", [128, 4, D], BF16)
            r_xs = [P.res("x1s%d" % j) for j in range(4)]
            stat = sbp("stat3", [128, 8], F32)
            r_stat = P.res("stat3")
            wqb = Buf(P, sbp, "wqb", [128, KC, 128], BF16, 2)
            pT = Buf(P, psp, "pT3", [128, 512], BF16, 2)
            qps = Buf(P, psp, "qps", [128, 512], F32, 2)
            ei = 0
            for s_ in range(2):
                for j in range(4):
                    tt = s_ * 4 + j
                    xt, xr = xf.next()
                    P.dma("sp", xt[:], x1_d[tt * 128:(tt + 1) * 128, :], w=[xr])
                    P.op("act", lambda e, xt=xt, j=j: e.activation(out=xs4[:, j, :], in_=xt[:], func=AF.Square,
                                                                accum_out=stat[:, j:j + 1]), r=[xr], w=[r_xs[j], r_stat])
                    P.op("act", lambda e, j=j: e.activation(out=stat[:, j:j + 1], in_=stat[:, j:j + 1], func=AF.Sqrt,
                                                         scale=1.0 / D, bias=EPS), r=[r_stat], w=[r_stat])
                    P.op("dve", lambda e, j=j: e.reciprocal(out=stat[:, 4 + j:5 + j], in_=stat[:, j:j + 1]),
                         r=[r_stat], w=[r_stat])
                    P.op("dve", lambda e, xt=xt, j=j: e.tensor_scalar(out=xs4[:, j, :], in0=xt[:], scalar1=stat[:, 4 + j:5 + j],
                                                                   scalar2=None, op0=ALU.mult), r=[xr, r_stat], w=[r_xs[j]])
                for kc in range(KC):
                    pt, pr = pT.next()
                    for j in range(4):
                        P.op("pe", lambda e, pt=pt, j=j, kc=kc: e.transpose(
                            out=pt[:, j * 128:(j + 1) * 128], in_=xs4[:, j, kc * 128:(kc + 1) * 128], identity=identb[:]),
                            r=[r_xs[j], r_identb], w=[pr], inc=(j == 3))
                    dst = hT[:, kc, s_ * 512:(s_ + 1) * 512]
                    if ei % 2 == 0:
                        P.op("dve", lambda e, pt=pt, dst=dst, kc=kc: e.tensor_scalar(
                            out=dst, in0=pt[:], scalar1=A2[:, kc:kc + 1], scalar2=B2[:, kc:kc + 1],
                            op0=ALU.mult, op1=ALU.add), r=[pr, r_A2, r_modcol], w=[r_hT[s_]])
                    else:
                        P.op("act", lambda e, pt=pt, dst=dst, kc=kc: e.activation(
                            out=dst, in_=pt[:], func=AF.Identity, scale=A2[:, kc:kc + 1], bias=B2[:, kc:kc + 1]),
                            r=[pr, r_A2, r_modcol], w=[r_hT[s_]])
                    ei += 1
            P.dma("sp", h2T_d[:, :, 0:512], hT[:, :, 0:512], r=[r_hT[0]])
            P.dma("sp", h2T_d[:, :, 512:1024], hT[:, :, 512:1024], r=[r_hT[1]])
            wq_v = wq.rearrange("(kc p) n -> p kc n", p=128)
            for cc in range(16):
                wt, wr = wqb.next()
                P.dma("pool", wt[:], wq_v[:, :, cc * 128:(cc + 1) * 128], w=[wr])
                for hf in range(2):
                    qt, qr = qps.next()
                    for kc in range(KC):
                        P.op("pe", lambda e, qt=qt, wt=wt, kc=kc, hf=hf: e.matmul(
                            qt[:], lhsT=wt[:, kc, :], rhs=hT[:, kc, hf * 512:(hf + 1) * 512],
                            start=(kc == 0), stop=(kc == KC - 1)), r=[wr, r_hT[hf]], w=[qr], inc=(kc == KC - 1))
                    P.op("act", lambda e, qt=qt, cc=cc, hf=hf: e.copy(out=qpT[:, cc, hf * 512:(hf + 1) * 512], in_=qt[:]),
                         r=[qr], w=[r_qp[hf]])
            P.barrier()
        with ExitStack() as ph:
            def sbp(name, shape, dt):
                return ph.enter_context(nc.sbuf_tensor(name, shape, dt))

            def psp(name, shape, dt):
                return ph.enter_context(nc.psum_tensor(name, shape, dt))

            skT = sbp("skT", [128, 16, 128], F32)
            iot = sbp("iot", [128, 128], F32)
            r_sk, r_iot = P.res("skT"), P.res("iot")
            P.dma("sp", skT[:], subkT, w=[r_sk])
            P.dma("sp", iot[:], iota_in, w=[r_iot])
            ssb = sbp("ssb", [128, 2048], F32)
            s2 = sbp("s2", [128, 2048], F32)
            cand = sbp("cand", [128, 8, 256], F32)
            cand2 = sbp("cand2", [128, 8, 256], F32)
            oh = sbp("oh", [128, 8, 256], F32)
            v16 = sbp("v16", [128, 16, 16], F32)
            i16 = sbp("i16", [128, 16, 16], U32)
            i16f = sbp("i16f", [128, 16, 16], F32)
            c16 = sbp("c16", [128, 8, 16], F32)
            p16 = sbp("p16", [128, 8, 16], U32)
            pa = sbp("pa", [128, 8, 16], U32)
            pb = sbp("pb", [128, 8, 16], U32)
            paf = sbp("paf", [128, 8, 16], F32)
            pbf = sbp("pbf", [128, 8, 16], F32)
            g16 = sbp("g16", [128, 8, 16], F32)
            e16 = sbp("e16", [128, 8, 16], F32)
            z8 = sbp("z8", [128, 8], F32)
            i0f = sbp("i0f", [128, 128], F32)
            i1f = sbp("i1f", [128, 128], F32)
            trT = sbp("trT", [128, 3, 128], F32)
            Bm = sbp("Bm", [128, 64, 128], BF16)
            gA = sbp("gA", [128, 64, 128], BF16)
            Gst = Buf(P, sbp, "Gst", [128, 128, 64], BF16, 2)
            r = {k: P.res(k) for k in ("ssb", "s2", "cand", "cand2", "oh", "v16", "i16", "c16", "p16", "pab", "g16",
                                       "i01", "trT", "Bm", "gA")}
            sps = Buf(P, psp, "sps", [128, 512], F32, 4)
            trps = Buf(P, psp, "trps", [128, 512], F32, 1)
            Gps = Buf(P, psp, "Gps", [128, 512], F32, 3)
            v16v = v16[:].rearrange("p (h two) k -> p h two k", two=2)
            i16v = i16f[:].rearrange("p (h two) k -> p h two k", two=2)
            for tt in range(NT):
                for qd in range(4):
                    st_, sr_ = sps.next()
                    for k in range(4):
                        hp = qd * 4 + k
                        P.op("pe", lambda e, st_=st_, k=k, hp=hp, tt=tt: e.matmul(
                            st_[:, k * 128:(k + 1) * 128], lhsT=qpT[:, hp, tt * 128:(tt + 1) * 128], rhs=skT[:, hp, :],
                            start=True, stop=True), r=[r_qp[tt // 4], r_sk], w=[sr_], inc=(k == 3))
                    P.op("act", lambda e, st_=st_, qd=qd: e.copy(out=ssb[:, qd * 512:(qd + 1) * 512], in_=st_[:]),
                         r=[sr_], w=[r["ssb"]])
                for hp in range(16):
                    sl = slice(hp * 128, (hp + 1) * 128)
                    P.op("dve", lambda e, hp=hp, sl=sl: e.max(out=v16[:, hp, 0:8], in_=ssb[:, sl]), r=[r["ssb"]], w=[r["v16"]])
                    P.op("dve", lambda e, hp=hp, sl=sl: e.max_index(out=i16[:, hp, 0:8], in_max=v16[:, hp, 0:8], in_values=ssb[:, sl]),
                         r=[r["ssb"], r["v16"]], w=[r["i16"]])
                    P.op("dve", lambda e, hp=hp, sl=sl: e.match_replace(out=s2[:, sl], in_to_replace=v16[:, hp, 0:8],
                                                                       in_values=ssb[:, sl], imm_value=NEG),
                         r=[r["ssb"], r["v16"]], w=[r["s2"]])
                    P.op("dve", lambda e, hp=hp, sl=sl: e.max(out=v16[:, hp, 8:16], in_=s2[:, sl]), r=[r["s2"]], w=[r["v16"]])
                    P.op("dve", lambda e, hp=hp, sl=sl: e.max_index(out=i16[:, hp, 8:16], in_max=v16[:, hp, 8:16], in_values=s2[:, sl]),
                         r=[r["s2"], r["v16"]], w=[r["i16"]])
                P.op("dve", lambda e: e.tensor_copy(out=i16f[:], in_=i16[:]), r=[r["i16"]], w=[r["i16"]])
                candv = cand[:].rearrange("p h (a b) -> p h a b", a=16)
                P.op("dve", lambda e: e.tensor_tensor(
                    out=cand[:].rearrange("p h (a b) -> p h a b", a=16)[:, 0:4],
                    in0=v16v[:, 0:4, 0, :].unsqueeze(3).to_broadcast([128, 4, 16, 16]),
                    in1=v16v[:, 0:4, 1, :].unsqueeze(2).to_broadcast([128, 4, 16, 16]), op=ALU.add),
                    r=[r["v16"]], w=[r["cand"]]) if False else None
                for h in range(8):
                    P.op("dve", lambda e, h=h: e.tensor_tensor(
                        out=cand[:, h, :].rearrange("p (a b) -> p a b", a=16),
                        in0=v16[:, 2 * h, :].unsqueeze(2).to_broadcast([128, 16, 16]),
                        in1=v16[:, 2 * h + 1, :].unsqueeze(1).to_broadcast([128, 16, 16]), op=ALU.add),
                        r=[r["v16"]], w=[r["cand"]])
                for h in range(8):
                    P.op("dve", lambda e, h=h: e.max(out=c16[:, h, 0:8], in_=cand[:, h, :]), r=[r["cand"]], w=[r["c16"]])
                    P.op("dve", lambda e, h=h: e.max_index(out=p16[:, h, 0:8], in_max=c16[:, h, 0:8], in_values=cand[:, h, :]),
                         r=[r["cand"], r["c16"]], w=[r["p16"]])
                    P.op("dve", lambda e, h=h: e.match_replace(out=cand2[:, h, :], in_to_replace=c16[:, h, 0:8],
                                                              in_values=cand[:, h, :], imm_value=NEG),
                         r=[r["cand"], r["c16"]], w=[r["cand2"]])
                    P.op("dve", lambda e, h=h: e.max(out=c16[:, h, 8:16], in_=cand2[:, h, :]), r=[r["cand2"]], w=[r["c16"]])
                    P.op("dve", lambda e, h=h: e.max_index(out=p16[:, h, 8:16], in_max=c16[:, h, 8:16], in_values=cand2[:, h, :]),
                         r=[r["cand2"], r["c16"]], w=[r["p16"]])
                P.op("dve", lambda e: e.tensor_tensor(out=e16[:], in0=c16[:], in1=c16[:, :, 0:1].to_broadcast([128, 8, 16]),
                                                      op=ALU.subtract), r=[r["c16"]], w=[r["g16"]])
                P.op("act", lambda e: e.activation(out=e16[:], in_=e16[:], func=AF.Exp), r=[r["g16"]], w=[r["g16"]])
                P.op("dve", lambda e: e.tensor_reduce(out=z8[:], in_=e16[:], axis=AX.X, op=ALU.add), r=[r["g16"]], w=[r["g16"]])
                P.op("dve", lambda e: e.reciprocal(out=z8[:], in_=z8[:]), r=[r["g16"]], w=[r["g16"]])
                P.op("dve", lambda e: e.tensor_tensor(out=g16[:], in0=e16[:], in1=z8[:].unsqueeze(2).to_broadcast([128, 8, 16]),
                                                      op=ALU.mult), r=[r["g16"]], w=[r["g16"]])
                P.op("dve", lambda e: e.tensor_single_scalar(out=pa[:], in_=p16[:], scalar=4, op=ALU.logical_shift_right),
                     r=[r["p16"]], w=[r["pab"]])
                P.op("dve", lambda e: e.tensor_single_scalar(out=pb[:], in_=p16[:], scalar=15, op=ALU.bitwise_and),
                     r=[r["p16"]], w=[r["pab"]])
                P.op("dve", lambda e: e.tensor_copy(out=paf[:], in_=pa[:]), r=[r["pab"]], w=[r["pab"]])
                P.op("dve", lambda e: e.tensor_copy(out=pbf[:], in_=pb[:]), r=[r["pab"]], w=[r["pab"]])
                for which, (pf, dstt) in enumerate(((paf, i0f), (pbf, i1f))):
                    for h in range(8):
                        ohv = oh[:, h, :].rearrange("p (k a) -> p k a", k=16)
                        P.op("dve", lambda e, h=h, pf=pf, ohv=ohv: e.tensor_tensor(
                            out=ohv, in0=pf[:, h, :].unsqueeze(2).to_broadcast([128, 16, 16]),
                            in1=iot[:, 0:16].unsqueeze(1).to_broadcast([128, 16, 16]), op=ALU.is_equal),
                            r=[r["pab"], r_iot], w=[r["oh"]])
                        P.op("dve", lambda e, h=h, which=which, ohv=ohv: e.tensor_tensor(
                            out=ohv, in0=ohv, in1=i16f[:, 2 * h + which, :].unsqueeze(1).to_broadcast([128, 16, 16]),
                            op=ALU.mult), r=[r["oh"], r["i16"]], w=[r["oh"]])
                    P.op("dve", lambda e, dstt=dstt: e.tensor_reduce(
                        out=dstt[:], in_=oh[:].rearrange("p h (k a) -> p (h k) a", k=16), axis=AX.X, op=ALU.add),
                        r=[r["oh"]], w=[r["i01"]])
                tp, tpr = trps.next()
                for w_, src in enumerate((i0f[:], i1f[:], g16[:].rearrange("p h k -> p (h k)"))):
                    P.op("pe", lambda e, tp=tp, w_=w_, src=src: e.transpose(
                        out=tp[:, w_ * 128:(w_ + 1) * 128], in_=src, identity=identf[:]),
                        r=[r["i01"], r["g16"], r_identf], w=[tpr], inc=(w_ == 2))
                P.op("act", lambda e, tp=tp: e.copy(out=trT[:].rearrange("p a t -> p (a t)"), in_=tp[:, 0:384]),
                     r=[tpr], w=[r["trT"]])
                for hf in range(2):
                    tsl = slice(hf * 64, (hf + 1) * 64)
                    iob = iot[:].unsqueeze(1).to_broadcast([128, 64, 128])
                    P.op("dve", lambda e, tsl=tsl, iob=iob: e.tensor_tensor(
                        out=Bm[:], in0=iob, in1=trT[:, 1, tsl].unsqueeze(2).to_broadcast([128, 64, 128]), op=ALU.is_equal),
                        r=[r_iot, r["trT"]], w=[r["Bm"]])
                    P.op("dve", lambda e, tsl=tsl, iob=iob: e.tensor_tensor(
                        out=gA[:], in0=iob, in1=trT[:, 0, tsl].unsqueeze(2).to_broadcast([128, 64, 128]), op=ALU.is_equal),
                        r=[r_iot, r["trT"]], w=[r["gA"]])
                    P.op("dve", lambda e, tsl=tsl: e.tensor_tensor(
                        out=gA[:], in0=gA[:], in1=trT[:, 2, tsl].unsqueeze(2).to_broadcast([128, 64, 128]), op=ALU.mult),
                        r=[r["gA"], r["trT"]], w=[r["gA"]])
                    gs, gsr = Gst.next()
                    for t4 in range(16):
                        gp, gpr = Gps.next()
                        for k in range(4):
                            tl = t4 * 4 + k
                            P.op("pe", lambda e, gp=gp, k=k, tl=tl: e.matmul(
                                gp[:, k * 128:(k + 1) * 128], lhsT=Bm[:, tl, :], rhs=gA[:, tl, :], start=True, stop=True),
                                r=[r["Bm"], r["gA"]], w=[gpr], inc=(k == 3))
                        src = gp[:].rearrange("p (t i) -> p i t", t=4)
                        dst = gs[:, :, t4 * 4:(t4 + 1) * 4]
                        if t4 % 2 == 0:
                            P.op("act", lambda e, src=src, dst=dst: e.copy(out=dst, in_=src), r=[gpr], w=[gsr])
                        else:
                            P.op("dve", lambda e, src=src, dst=dst: e.tensor_copy(out=dst, in_=src), r=[gpr], w=[gsr])
                    t0 = tt * 128 + hf * 64
                    for i8 in range(8):
                        P.dma("sp", GT_d[i8 * 16:(i8 + 1) * 16, :, t0:t0 + 64].rearrange("i j t -> j i t"),
                              gs[:, i8 * 16:(i8 + 1) * 16, :], r=[gsr])
            P.barrier()


def phase4(nc, P, L):
    uT, vtab, h2T_d, GT_d, actg_d, y_d, x1_d, mod_d, lnf_row, out_d = (L[k] for k in
        ("uT", "vtab", "h2T_d", "GT_d", "actg_d", "y_d", "x1_d", "mod_d", "lnf_row", "out_d"))
    NP = 64
    with ExitStack() as ph4:
        ssq = ph4.enter_context(nc.sbuf_tensor("ssq4", [128, 2, NT], F32))
        r_ssq = P.res("ssq4")
        for half in range(2):
            with ExitStack() as ph:
                def sbp(name, shape, dt):
                    return ph.enter_context(nc.sbuf_tensor(name, shape, dt))

                def psp(name, shape, dt):
                    return ph.enter_context(nc.psum_tensor(name, shape, dt))

                acc = sbp("acc%d" % half, [128, NT, 2048], F32)
                r_acc = [[P.res("acc_%d_%d" % (tt, ds)) for ds in range(4)] for tt in range(NT)]
                for tt in range(NT):
                    P.op("pool", lambda e, tt=tt: e.memset(acc[:, tt, :], 0.0), w=r_acc[tt])
                phm = ExitStack()

                def sbm(name, shape, dt):
                    return phm.enter_context(nc.sbuf_tensor(name, shape, dt))

                if half == 0:
                    hT = sbm("h2Tr", [128, KC, T], BF16)
                    r_hT = P.res("h2Tr")
                    P.dma("sp", hT[:, 0:16, :], h2T_d[:, 0:16, :], w=[r_hT])
                    P.dma("sp", hT[:, 16:32, :], h2T_d[:, 16:32, :], w=[r_hT])
                    ub = Buf(P, sbm, "ub", [128, KC, 256], BF16, 2)
                    gtb = Buf(P, sbm, "gtb", [128, 2, T], BF16, 2)
                    aps = Buf(P, psp, "aps", [128, 512], F32, 4)
                GB = 2 if half == 0 else 8
                NG = 128 // GB
                vb_ = Buf(P, sbm, "vb%d" % half, [128, GB, 2048], BF16, 2)
                ab = Buf(P, sbm, "ab%d" % half, [128, GB, T], BF16, 2)
                ops_ = Buf(P, psp, "ops%d" % half, [128, 512], F32, 4)
                uT_v = uT.rearrange("(kc p) e -> p kc e", p=128)
                d0 = half * 2048

                def prefetch(p):
                    res = {}
                    if half == 0:
                        ut, ur = ub.next()
                        P.dma("pool", ut[:], uT_v[:, :, p * 256:(p + 1) * 256], w=[ur])
                        gt, gr = gtb.next()
                        P.dma("sp", gt[:], GT_d[2 * p:2 * p + 2].rearrange("i j t -> j i t"), w=[gr])
                        res["u"] = (ut, ur)
                        res["g"] = (gt, gr)
                    vt, vr = vb_.next()
                    for b2 in range(0, GB, 2):
                        P.dma("pool", vt[:, b2:b2 + 2, :],
                              vtab[(p * GB + b2) * 128:(p * GB + b2 + 2) * 128, d0:d0 + 2048].rearrange("(b e) d -> e b d", b=2),
                              w=[vr])
                    res["v"] = (vt, vr)
                    at, ar = ab.next()
                    if half == 1:
                        for b2 in range(0, GB, 2):
                            P.dma("sp", at[:, b2:b2 + 2, :], actg_d[p * GB + b2:p * GB + b2 + 2].rearrange("b e t -> e b t"), w=[ar])
                    res["a"] = (at, ar)
                    return res

                nxt = prefetch(0)
                for p in range(NG):
                    cur = nxt
                    if p + 1 < NG:
                        nxt = prefetch(p + 1)
                    at, ar = cur["a"]
                    vt, vr = cur["v"]
                    if half == 0:
                        ut, ur = cur["u"]
                        gt, gr = cur["g"]
                        for b in range(2):
                            for hf in range(2):
                                pt, pr = aps.next()
                                for kc in range(KC):
                                    P.op("pe", lambda e, pt=pt, ut=ut, kc=kc, b=b, hf=hf: e.matmul(
                                        pt[:], lhsT=ut[:, kc, b * 128:(b + 1) * 128], rhs=hT[:, kc, hf * 512:(hf + 1) * 512],
                                        start=(kc == 0), stop=(kc == KC - 1)), r=[ur, r_hT], w=[pr], inc=(kc == KC - 1))
                                P.op("act", lambda e, pt=pt, at=at, b=b, hf=hf: e.activation(
                                    out=at[:, b, hf * 512:(hf + 1) * 512], in_=pt[:], func=AF.Gelu), r=[pr], w=[ar])
                        P.op("dve", lambda e, at=at, gt=gt: e.tensor_tensor(
                            out=at[:].rearrange("p b t -> p (b t)"), in0=at[:].rearrange("p b t -> p (b t)"),
                            in1=gt[:].rearrange("p b t -> p (b t)"), op=ALU.mult), r=[ar, gr], w=[ar])
                        P.dma("sp", actg_d[2 * p:2 * p + 2].rearrange("b e t -> e b t"), at[:], r=[ar])
                    for tt in range(NT):
                        for ds in range(4):
                            ot, orr = ops_.next()
                            for b in range(GB):
                                P.op("pe", lambda e, ot=ot, at=at, vt=vt, b=b, tt=tt, ds=ds: e.matmul(
                                    ot[:], lhsT=at[:, b, tt * 128:(tt + 1) * 128], rhs=vt[:, b, ds * 512:(ds + 1) * 512],
                                    start=(b == 0), stop=(b == GB - 1)), r=[ar, vr], w=[orr], inc=(b == GB - 1))
                            dst = acc[:, tt, ds * 512:(ds + 1) * 512]
                            P.op("dve", lambda e, ot=ot, dst=dst: e.tensor_tensor(out=dst, in0=ot[:], in1=dst, op=ALU.add),
                                 r=[orr, r_acc[tt][ds]], w=[r_acc[tt][ds]])
                P.barrier()
                phm.close()
                g2 = sbp("g2bc%d" % half, [128, 2048], F32)
                r_g2 = P.res("g2bc")
                P.dma("sp", g2[:], mod_d[5 * D + d0:5 * D + d0 + 2048].partition_broadcast(128), w=[r_g2])
                xr_ = Buf(P, sbp, "x1r%d" % half, [128, 2048], F32, 2)
                for tt in range(NT):
                    xt, xr = xr_.next()
                    P.dma("sp", xt[:], x1_d[tt * 128:(tt + 1) * 128, d0:d0 + 2048], w=[xr])
                    P.op("dve", lambda e, tt=tt: e.tensor_tensor(out=acc[:, tt, :], in0=acc[:, tt, :], in1=g2[:], op=ALU.mult),
                         r=r_acc[tt] + [r_g2], w=r_acc[tt])
                    P.op("pool", lambda e, tt=tt, xt=xt: e.tensor_tensor(out=acc[:, tt, :], in0=acc[:, tt, :], in1=xt[:], op=ALU.add),
                         r=r_acc[tt] + [xr], w=r_acc[tt])
                    P.op("act", lambda e, tt=tt, xt=xt, half=half: e.activation(
                        out=xt[:], in_=acc[:, tt, :], func=AF.Square, accum_out=ssq[:, half, tt:tt + 1]),
                        r=r_acc[tt], w=[xr, r_ssq])
                    P.dma("sp", y_d[tt * 128:(tt + 1) * 128, d0:d0 + 2048], acc[:, tt, :], r=r_acc[tt])
                P.barrier()
        with ExitStack() as ph:
            def sbp(name, shape, dt):
                return ph.enter_context(nc.sbuf_tensor(name, shape, dt))

            lnf = sbp("lnfbc", [128, D], F32)
            r_lnf = P.res("lnfbc")
            P.dma("sp", lnf[:], lnf_row.partition_broadcast(128), w=[r_lnf])
            rs = sbp("rsf", [128, NT], F32)
            P.op("dve", lambda e: e.tensor_tensor(out=rs[:], in0=ssq[:, 0, :], in1=ssq[:, 1, :], op=ALU.add), r=[r_ssq], w=[r_ssq])
            P.op("act", lambda e: e.activation(out=rs[:], in_=rs[:], func=AF.Sqrt, scale=1.0 / D, bias=EPS), r=[r_ssq], w=[r_ssq])
            P.op("dve", lambda e: e.reciprocal(out=rs[:], in_=rs[:]), r=[r_ssq], w=[r_ssq])
            yb = Buf(P, sbp, "yfin", [128, D], F32, 2)
            for tt in range(NT):
                yt, yr = yb.next()
                P.dma("sp", yt[:], y_d[tt * 128:(tt + 1) * 128, :], w=[yr])
                P.op("dve", lambda e, yt=yt, tt=tt: e.scalar_tensor_tensor(
                    out=yt[:], in0=yt[:], scalar=rs[:, tt:tt + 1], in1=lnf[:], op0=ALU.mult, op1=ALU.mult),
                    r=[yr, r_ssq, r_lnf], w=[yr])
                P.dma("sp", out_d[tt * 128:(tt + 1) * 128, :], yt[:], r=[yr])
            P.barrier()


_NC_CACHE = {}


def kernel(**inputs):
    stage = 4
    if "nc" not in _NC_CACHE:
        _NC_CACHE["nc"] = build(stage=stage)
    nc = _NC_CACHE["nc"]
    shared = prep_shared(inputs, stage)
    in_maps = []
    for core in range(8):
        b, q = core // 4, core % 4
        in_maps.append(prep_core(inputs, shared, b, q))
    res = run_bass_kernel_spmd(nc, in_maps, core_ids=list(range(8)))
    out = np.zeros((2, 4096, D), dtype=np.float32)
    for core in range(8):
        b, q = core // 4, core % 4
        out[b, q::4, :] = np.asarray(res.results[core]["out"], dtype=np.float32)
    return out
```

```python
import math
from contextlib import ExitStack

import numpy as np
import concourse.bass as bass
import concourse.mybir as mybir
from concourse.bass_utils import run_bass_kernel_spmd

F32 = mybir.dt.float32
BF16 = mybir.dt.bfloat16
I32 = mybir.dt.int32
U32 = mybir.dt.uint32
AF = mybir.ActivationFunctionType
ALU = mybir.AluOpType
AX = mybir.AxisListType

D = 4096
T = 1024
NT = 8
KC = 32
IN_COLS = 6480
EPS = 1e-6
NEG = -1.0e30
TWO_PI = 2.0 * math.pi


class Res:
    __slots__ = ("name", "w", "rs")

    def __init__(self, name):
        self.name = name
        self.w = None
        self.rs = []


class Prog:
    ENGS = ("pe", "act", "dve", "pool", "sp")

    def __init__(self, nc, es, n_dma_sems=8):
        self.nc = nc
        self.ops = {e: [] for e in self.ENGS}
        self.sem = {}
        self.cnt = {}
        for e in ("pe", "act", "dve", "pool"):
            self.sem[e] = es.enter_context(nc.semaphore("s_" + e))
            self.cnt[e] = 0
        self.dsem = {}
        self.drr = {}
        for q in ("sp", "pool", "act"):
            self.dsem[q] = []
            for k in range(n_dma_sems):
                key = "d_%s%d" % (q, k)
                self.sem[key] = es.enter_context(nc.semaphore(key))
                self.cnt[key] = 0
                self.dsem[q].append(key)
            self.drr[q] = 0
        self.seen = {e: {} for e in self.ENGS}
        self.pend = {e: ([], []) for e in self.ENGS}
        self.nres = 0

    def res(self, name=None):
        self.nres += 1
        return Res(name or ("r%d" % self.nres))

    def _waits(self, e, r, w):
        need = {}

        def add(ref, kind):
            if ref is None:
                return
            key, val, src = ref
            if src == e and (e == "pe" or kind == "war"):
                return
            if need.get(key, 0) < val:
                need[key] = val

        for x in r:
            add(x.w, "raw")
        for x in w:
            add(x.w, "waw")
            for ref in x.rs:
                add(ref, "war")
        out = []
        seen = self.seen[e]
        for key, val in need.items():
            if seen.get(key, 0) < val:
                seen[key] = val
                out.append((key, val))
        return out

    @staticmethod
    def _commit(ref, r, w):
        for x in r:
            x.rs.append(ref)
        for x in w:
            x.w = ref
            x.rs = []

    def op(self, e, fn, r=(), w=(), inc=True):
        r = list(r)
        w = list(w)
        waits = self._waits(e, r, w)
        if not inc:
            self.pend[e][0].extend(r)
            self.pend[e][1].extend(w)
            self.ops[e].append((waits, fn, None))
            return None
        self.cnt[e] += 1
        ref = (e, self.cnt[e], e)
        pr, pw = self.pend[e]
        self._commit(ref, r + pr, w + pw)
        self.pend[e] = ([], [])
        self.ops[e].append((waits, fn, (e, 1)))
        return ref

    def dma(self, q, out, in_, r=(), w=(), **kw):
        r = list(r)
        w = list(w)
        key = self.dsem[q][self.drr[q] % len(self.dsem[q])]
        self.drr[q] += 1
        waits = self._waits(q, r, w)
        prev = self.cnt[key]
        if prev and self.seen[q].get(key, 0) < prev:
            self.seen[q][key] = prev
            waits.append((key, prev))
        self.cnt[key] += 16
        ref = (key, self.cnt[key], "dma_" + q)
        self._commit(ref, r, w)

        def fn(eng, out=out, in_=in_, kw=kw):
            return eng.dma_start(out=out, in_=in_, **kw)

        self.ops[q].append((waits, fn, (key, 16)))
        return ref

    def barrier(self):
        for e in self.ENGS:
            waits = []
            for key, val in self.cnt.items():
                if val and self.seen[e].get(key, 0) < val:
                    self.seen[e][key] = val
                    waits.append((key, val))
            self.ops[e].append((waits, None, None))

    def emit(self):
        nc = self.nc
        sem = self.sem

        def mk(e):
            def body(eng):
                for waits, fn, inc in self.ops[e]:
                    for key, val in waits:
                        eng.wait_ge(sem[key], val)
                    if fn is None:
                        continue
                    ins = fn(eng)
                    if inc is not None:
                        ins.then_inc(sem[inc[0]], inc[1])
            return body

        with nc.Block() as block:
            block.tensor(mk("pe"))
            block.scalar(mk("act"))
            block.vector(mk("dve"))
            block.gpsimd(mk("pool"))
            block.sync(mk("sp"))


class Buf:
    def __init__(self, P, alloc, name, shape, dt, n=1):
        self.t = [alloc("%s%d" % (name, i), shape, dt) for i in range(n)]
        self.r = [P.res("%s%d" % (name, i)) for i in range(n)]
        self.n = n
        self.i = -1

    def next(self):
        self.i = (self.i + 1) % self.n
        return self.t[self.i], self.r[self.i]

    def cur(self):
        return self.t[self.i], self.r[self.i]


def proj_chunks(own):
    ch = []
    if own:
        for c0 in range(0, 2048, 256):
            ch.append((c0, 256, "qa"))
    ch.append((2048, 256, "ka"))
    ch.append((2304, 256, "ka"))
    ch.append((2560, 256, "va"))
    ch.append((2816, 256, "va"))
    if own:
        for c0 in range(3072, 5120, 256):
            ch.append((c0, 256, "qb"))
    ch.append((5120, 256, "kbvb"))
    if own:
        for c0 in range(5376, 6400, 256):
            ch.append((c0, 256, "qi"))
    ch.append((6400, 80, "kiwi"))
    return ch


def build(stage=99, debug=False, only=None, scratch_in=()):
    nc = bass.Bass("TRN2", target_bir_lowering=False)

    def want(name):
        return only is None or name in only

    def din(name, shape, dt=F32):
        return nc.dram_tensor(name, shape, dt, kind="ExternalInput").ap()

    def dscr(name, shape, dt):
        if name in scratch_in:
            return nc.dram_tensor(name, shape, dt, kind="ExternalInput").ap()
        if debug:
            return nc.dram_tensor(name, shape, dt, kind="ExternalOutput").ap()
        return nc.dram_tensor(name, shape, dt).ap()

    xg = din("xg", [4, T, D]) if (want("p1") or want("p2c")) else None
    posT = din("posT", [128, 32], I32)
    c_col = din("c_col", [128, KC])
    ln1_col = din("ln1_col", [128, KC])
    ln2_col = din("ln2_col", [128, KC])
    lnf_row = din("lnf_row", [D])
    b_ada = din("b_ada", [6 * D])
    w_ada = din("w_ada", [D, 6 * D]) if want("p0") else None
    w_in = din("w_in", [D, IN_COLS]) if want("p1") else None
    w_out = din("w_out", [D, D]) if (stage >= 3 and want("p2c")) else None
    wq = din("wq", [D, 2048]) if (stage >= 4 and want("p3")) else None
    subkT = din("subkT", [128, 16, 128])
    if stage >= 4 and want("p4"):
        uT = din("uT", [D, 16384])
        vtab = din("vtab", [16384, D])
    ident_in = din("ident", [128, 128])
    inv_in = din("inv_bc", [128, 96])
    iota_in = din("iota128", [128, 128])
    maskA_in = din("maskA", [128, 15, 128])
    cmaskB_in = din("cmaskB", [128, 4, 128])
    out_d = nc.dram_tensor("out", [T, D], F32, kind="ExternalOutput").ap()

    mod_d = dscr("mod_d", [6 * D], F32)
    qT_d = dscr("qT_d", [32, 128, T], BF16)
    kTa_d = dscr("kTa_d", [4, 128, 4096], BF16)
    va_d = dscr("va_d", [4096, 512], BF16)
    kTb_d = dscr("kTb_d", [128, 4096], BF16)
    vb_d = dscr("vb_d", [4096, 128], BF16)
    kiT_d = dscr("kiT_d", [128, 4096], BF16)
    qiT_d = dscr("qiT_d", [8, 128, T], BF16)
    wi_d = dscr("wi_d", [T, 16], F32)
    oT_d = dscr("oT_d", [32, 128, T], BF16)
    x1_d = dscr("x1_d", [T, D], F32)
    h2T_d = dscr("h2T_d", [128, KC, T], BF16)
    GT_d = dscr("GT_d", [128, 128, T], BF16)
    actg_d = dscr("actg_d", [128, 128, T], BF16)
    y_d = dscr("y_d", [T, D], F32)
    ssq_d = dscr("ssq_d", [2, T], F32)

    with ExitStack() as es:
        P = Prog(nc, es)

        def sb(name, shape, dt):
            return es.enter_context(nc.sbuf_tensor(name, shape, dt))

        identf = sb("identf", [128, 128], F32)
        identb = sb("identb", [128, 128], BF16)
        modcol = sb("modcol", [128, 192], F32)
        A1 = sb("A1", [128, KC], F32)
        A2 = sb("A2", [128, KC], F32)
        ln1c = sb("ln1c", [128, KC], F32)
        ln2c = sb("ln2c", [128, KC], F32)
        ones_f = sb("ones_f", [128, 128], F32)
        r_identf, r_identb, r_modcol, r_A1, r_A2, r_ln, r_ones = (P.res(n) for n in
            ("identf", "identb", "modcol", "A1", "A2", "ln", "ones"))
        P.dma("sp", identf[:], ident_in, w=[r_identf])
        P.dma("sp", ln1c[:], ln1_col, w=[r_ln])
        P.dma("sp", ln2c[:], ln2_col, w=[r_ln])
        P.op("dve", lambda e: e.tensor_copy(out=identb[:], in_=identf[:]), r=[r_identf], w=[r_identb])
        P.op("pool", lambda e: e.memset(ones_f[:], 1.0), w=[r_ones])

        with ExitStack() as ph:
            def sbp(name, shape, dt):
                return ph.enter_context(nc.sbuf_tensor(name, shape, dt))

            def psp(name, shape, dt):
                return ph.enter_context(nc.psum_tensor(name, shape, dt))

            ccol = sbp("ccol", [128, KC], F32)
            scol = sbp("scol", [128, KC], BF16)
            r_c, r_s = P.res("ccol"), P.res("scol")
            P.dma("sp", ccol[:], c_col, w=[r_c])
            P.op("act", lambda e: e.activation(out=scol[:], in_=ccol[:], func=AF.Silu), r=[r_c], w=[r_s])
            wbuf = Buf(P, sbp, "wada", [128, KC, 512], BF16, 2)
            brow = Buf(P, sbp, "brow", [1, 512], F32, 2)
            rowsb = Buf(P, sbp, "rowsb", [1, 512], F32, 2)
            rowps = Buf(P, psp, "rowps", [1, 512], F32, 2)
            colps = Buf(P, psp, "colps", [128, 512], F32, 2)
            w_ada_v = w_ada.rearrange("(kc p) n -> p kc n", p=128) if w_ada is not None else None
            for j in (range(48) if want("p0") else ()):
                wt, wr = wbuf.next()
                P.dma("pool", wt[:], w_ada_v[:, :, j * 512:(j + 1) * 512], w=[wr])
                bt, br = brow.next()
                P.dma("sp", bt[:], b_ada[j * 512:(j + 1) * 512].unsqueeze(0), w=[br])
                pt, pr = rowps.next()
                for kc in range(KC):
                    P.op("pe", lambda e, pt=pt, wt=wt, kc=kc: e.matmul(
                        pt[:], lhsT=scol[:, kc:kc + 1], rhs=wt[:, kc, :],
                        start=(kc == 0), stop=(kc == KC - 1)),
                        r=[r_s, wr], w=[pr], inc=(kc == KC - 1))
                st, sr = rowsb.next()
                P.op("dve", lambda e, st=st, pt=pt, bt=bt: e.tensor_tensor(
                    out=st[:], in0=pt[:], in1=bt[:], op=ALU.add), r=[pr, br], w=[sr])
                P.dma("sp", mod_d[j * 512:(j + 1) * 512].unsqueeze(0), st[:], r=[sr])
                ct, cr = colps.next()
                for k in range(4):
                    P.op("pe", lambda e, ct=ct, st=st, k=k: e.matmul(
                        ct[:, k:k + 1], lhsT=st[0:1, k * 128:(k + 1) * 128], rhs=ones_f[0:1, 0:1],
                        start=True, stop=True), r=[sr, r_ones], w=[cr], inc=(k == 3))
                P.op("act", lambda e, ct=ct, j=j: e.copy(out=modcol[:, j * 4:(j + 1) * 4], in_=ct[:, 0:4]),
                     r=[cr], w=[r_modcol])
            P.op("dve", lambda e: e.scalar_tensor_tensor(
                out=A1[:], in0=modcol[:, 32:64], scalar=1.0, in1=ln1c[:], op0=ALU.add, op1=ALU.mult),
                r=[r_modcol, r_ln], w=[r_A1])
            P.op("dve", lambda e: e.scalar_tensor_tensor(
                out=A2[:], in0=modcol[:, 128:160], scalar=1.0, in1=ln2c[:], op0=ALU.add, op1=ALU.mult),
                r=[r_modcol, r_ln], w=[r_A2])
            P.barrier()
        B1 = modcol[:, 0:32]
        B2 = modcol[:, 96:128]

        if stage >= 1 and want("p1"):
            phase1(nc, P, locals())
        if stage >= 2 and want("p2a"):
            phase2a(nc, P, locals())
        if stage >= 2 and want("p2b"):
            phase2b(nc, P, locals())
        if stage >= 3 and want("p2c"):
            phase2c(nc, P, locals())
        if stage >= 4 and want("p3"):
            phase3(nc, P, locals())
        if stage >= 4 and want("p4"):
            phase4(nc, P, locals())

        P.barrier()
        P.emit()
    return nc


def phase1(nc, P, L):
    xg, posT, w_in = L["xg"], L["posT"], L["w_in"]
    identb, A1, B1 = L["identb"], L["A1"], L["B1"]
    r_identb, r_A1, r_modcol = L["r_identb"], L["r_A1"], L["r_modcol"]
    qT_d, kTa_d, va_d, kTb_d, vb_d, kiT_d, qiT_d, wi_d = (L[k] for k in
        ("qT_d", "kTa_d", "va_d", "kTb_d", "vb_d", "kiT_d", "qiT_d", "wi_d"))
    inv_in = L["inv_in"]
    with ExitStack() as ph:
        def sbp(name, shape, dt):
            return ph.enter_context(nc.sbuf_tensor(name, shape, dt))

        def psp(name, shape, dt):
            return ph.enter_context(nc.psum_tensor(name, shape, dt))

        hT = sbp("hT", [128, KC, T], BF16)
        r_hT = [P.res("hT%d" % s) for s in range(2)]
        xf = Buf(P, sbp, "xf", [128, D], F32, 2)
        xs4 = sbp("xs4", [128, 4, D], BF16)
        r_xs = [P.res("xs%d" % j) for j in range(4)]
        stat = sbp("stat", [128, 8], F32)
        r_stat = P.res("stat")
        wch = Buf(P, sbp, "wch", [128, KC, 256], BF16, 2)
        invb = sbp("invb", [128, 96], F32)
        posi = sbp("posi", [128, 32], I32)
        posf = sbp("posf", [128, 32], F32)
        cosT = sbp("cosT", [128, NT, 96], F32)
        sinT = sbp("sinT", [128, NT, 96], F32)
        nsinT = sbp("nsinT", [128, NT, 96], F32)
        r_tab = [P.res("tab%d" % i) for i in range(NT)]
        tA = sbp("tA", [128, 96], F32)
        tB = sbp("tB", [128, 96], F32)
        tC = sbp("tC", [128, 96], F32)
        tK = sbp("tK", [128, 96], I32)
        r_t = P.res("ttmp")
        r_inv, r_pos = P.res("inv"), P.res("pos")
        tmp1 = Buf(P, sbp, "tmp1", [128, 256], F32, 2)
        tmp2 = Buf(P, sbp, "tmp2", [128, 256], F32, 2)
        obf = Buf(P, sbp, "obf", [128, 256], BF16, 2)
        stT = Buf(P, sbp, "stT", [128, 2, T], BF16, 2)
        stV = Buf(P, sbp, "stV", [128, NT, 256], BF16, 2)
        wist = sbp("wist", [128, NT, 16], F32)
        r_wist = P.res("wist")
        pT = Buf(P, psp, "pT", [128, 512], BF16, 2)
        pp = Buf(P, psp, "pp", [128, 256], F32, 2)
        pq = Buf(P, psp, "pq", [128, 256], BF16, 2)

        P.dma("sp", invb[:], inv_in, w=[r_inv])
        P.dma("sp", posi[:], posT, w=[r_pos])
        P.op("pool", lambda e: e.tensor_copy(out=posf[:], in_=posi[:]), r=[r_pos], w=[r_pos])

        def rope_tables(gt):
            rt = r_tab[gt % NT]
            P.op("pool", lambda e: e.tensor_scalar(out=tA[:], in0=invb[:], scalar1=posf[:, gt:gt + 1],
                                                   scalar2=None, op0=ALU.mult), r=[r_inv, r_pos], w=[r_t])
            P.op("pool", lambda e: e.tensor_scalar(out=tK[:], in0=tA[:], scalar1=1.0 / TWO_PI,
                                                   scalar2=None, op0=ALU.mult), r=[r_t], w=[r_t])
            P.op("pool", lambda e: e.tensor_copy(out=tB[:], in_=tK[:]), r=[r_t], w=[r_t])
            P.op("pool", lambda e: e.tensor_scalar(out=tB[:], in0=tB[:], scalar1=-TWO_PI,
                                                   scalar2=None, op0=ALU.mult), r=[r_t], w=[r_t])
            P.op("pool", lambda e: e.tensor_tensor(out=tA[:], in0=tA[:], in1=tB[:], op=ALU.add), r=[r_t], w=[r_t])

            def wrap(dst, shift):
                P.op("pool", lambda e: e.tensor_scalar(out=dst[:], in0=tA[:], scalar1=shift, scalar2=None,
                                                       op0=ALU.add), r=[r_t], w=[r_t])
                P.op("pool", lambda e: e.tensor_scalar(out=tB[:], in0=dst[:], scalar1=math.pi, scalar2=-TWO_PI,
                                                       op0=ALU.is_gt, op1=ALU.mult), r=[r_t], w=[r_t])
                P.op("pool", lambda e: e.tensor_tensor(out=dst[:], in0=dst[:], in1=tB[:], op=ALU.add), r=[r_t], w=[r_t])
                P.op("pool", lambda e: e.tensor_scalar(out=tB[:], in0=dst[:], scalar1=-math.pi, scalar2=TWO_PI,
                                                       op0=ALU.is_lt, op1=ALU.mult), r=[r_t], w=[r_t])
                P.op("pool", lambda e: e.tensor_tensor(out=dst[:], in0=dst[:], in1=tB[:], op=ALU.add), r=[r_t], w=[r_t])

            wrap(tC, 0.0)
            P.op("act", lambda e: e.activation(out=sinT[:, gt % NT, :], in_=tC[:], func=AF.Sin), r=[r_t], w=[rt])
            P.op("act", lambda e: e.activation(out=nsinT[:, gt % NT, :], in_=tC[:], func=AF.Sin, scale=-1.0), r=[r_t], w=[rt])
            wrap(tC, 0.5 * math.pi)
            P.op("act", lambda e: e.activation(out=cosT[:, gt % NT, :], in_=tC[:], func=AF.Sin), r=[r_t], w=[rt])

        def rope(ppt, ppr, gt, H, half, t0, ocols, cols):
            rt = r_tab[gt % NT]
            gt = gt % NT
            n = H * 2 * half
            t1, r1 = tmp1.next()
            t2, r2 = tmp2.next()
            ot, orr = obf.cur()
            src = ppt[:, cols[0]:cols[0] + n].rearrange("p (h two f) -> p h two f", h=H, two=2)
            cosb = cosT[:, gt, t0:t0 + half].unsqueeze(1).unsqueeze(1).to_broadcast([128, H, 2, half])
            sinb = sinT[:, gt, t0:t0 + half].unsqueeze(1).to_broadcast([128, H, half])
            nsinb = nsinT[:, gt, t0:t0 + half].unsqueeze(1).to_broadcast([128, H, half])
            t1v = t1[:, 0:n].rearrange("p (h two f) -> p h two f", h=H, two=2)
            t2v = t2[:, 0:n].rearrange("p (h two f) -> p h two f", h=H, two=2)
            P.op("dve", lambda e: e.tensor_tensor(out=t1v, in0=src, in1=cosb, op=ALU.mult), r=[ppr, rt], w=[r1])
            P.op("dve", lambda e: e.tensor_tensor(out=t2v[:, :, 0, :], in0=src[:, :, 1, :], in1=nsinb, op=ALU.mult),
                 r=[ppr, rt], w=[r2])
            P.op("dve", lambda e: e.tensor_tensor(out=t2v[:, :, 1, :], in0=src[:, :, 0, :], in1=sinb, op=ALU.mult),
                 r=[ppr, rt], w=[r2])
            P.op("dve", lambda e: e.tensor_tensor(out=ot[:, ocols:ocols + n], in0=t1[:, 0:n], in1=t2[:, 0:n], op=ALU.add),
                 r=[r1, r2], w=[orr])

        w_in_v = w_in.rearrange("(kc p) n -> p kc n", p=128)
        evac_i = [0]
        def norm(g, s):
            for j in range(4):
                tt = s * 4 + j
                gt = g * 8 + tt
                xt, xr = xf.next()
                P.dma("sp", xt[:], xg[g, tt * 128:(tt + 1) * 128, :], w=[xr])
                P.op("act", lambda e, xt=xt, j=j: e.activation(out=xs4[:, j, :], in_=xt[:], func=AF.Square,
                                                            accum_out=stat[:, j:j + 1]),
                     r=[xr], w=[r_xs[j], r_stat])
                P.op("act", lambda e, j=j: e.activation(out=stat[:, j:j + 1], in_=stat[:, j:j + 1], func=AF.Sqrt,
                                                     scale=1.0 / D, bias=EPS),
                     r=[r_stat], w=[r_stat])
                P.op("dve", lambda e, j=j: e.reciprocal(out=stat[:, 4 + j:5 + j], in_=stat[:, j:j + 1]),
                     r=[r_stat], w=[r_stat])
                P.op("dve", lambda e, xt=xt, j=j: e.tensor_scalar(out=xs4[:, j, :], in0=xt[:], scalar1=stat[:, 4 + j:5 + j],
                                                               scalar2=None, op0=ALU.mult),
                     r=[xr, r_stat], w=[r_xs[j]])
                rope_tables(gt)

        def trans(g, s):
            for kc in range(KC):
                pt, pr = pT.next()
                for j in range(4):
                    P.op("pe", lambda e, pt=pt, j=j, kc=kc: e.transpose(
                        out=pt[:, j * 128:(j + 1) * 128], in_=xs4[:, j, kc * 128:(kc + 1) * 128], identity=identb[:]),
                        r=[r_xs[j], r_identb], w=[pr], inc=(j == 3))
                dst = hT[:, kc, s * 512:(s + 1) * 512]
                if evac_i[0] % 2 == 0:
                    P.op("dve", lambda e, pt=pt, dst=dst, kc=kc: e.tensor_scalar(
                        out=dst, in0=pt[:], scalar1=A1[:, kc:kc + 1], scalar2=B1[:, kc:kc + 1],
                        op0=ALU.mult, op1=ALU.add), r=[pr, r_A1, r_modcol], w=[r_hT[s]])
                else:
                    P.op("act", lambda e, pt=pt, dst=dst, kc=kc: e.activation(
                        out=dst, in_=pt[:], func=AF.Identity, scale=A1[:, kc:kc + 1], bias=B1[:, kc:kc + 1]),
                        r=[pr, r_A1, r_modcol], w=[r_hT[s]])
                evac_i[0] += 1

        def mm(g, s):
            own = (g == 0)
            for (c0, wc, kind) in proj_chunks(own):
                wt, wr = wch.next()
                P.dma("pool", wt[:, :, 0:wc], w_in_v[:, :, c0:c0 + wc], w=[wr])
                if kind in ("qa", "qb", "ka", "qi"):
                    sT, sTr = stT.next()
                if kind in ("va",):
                    sV, sVr = stV.next()
                if kind == "kbvb":
                    sT, sTr = stT.next()
                    sV, sVr = stV.next()
                if kind == "kiwi":
                    sT, sTr = stT.next()
                post = [None]
                for tt in range(s * 4, s * 4 + 4):
                    gt = g * 8 + tt
                    ppt, ppr = pp.next()
                    for kc in range(KC):
                        P.op("pe", lambda e, ppt=ppt, wt=wt, kc=kc, tt=tt, wc=wc: e.matmul(
                            ppt[:, 0:wc], lhsT=hT[:, kc, tt * 128:(tt + 1) * 128], rhs=wt[:, kc, 0:wc],
                            start=(kc == 0), stop=(kc == KC - 1)),
                            r=[r_hT[tt // 4], wr], w=[ppr], inc=(kc == KC - 1))
                    if post[0] is not None:
                        post[0]()
                        post[0] = None
                    if kind in ("qa", "qb", "ka"):
                        obf.next()
                        rope(ppt, ppr, gt, 2, 64, 0, 0, (0,))
                        ot, orr = obf.cur()

                        def pst(ot=ot, orr=orr, sT=sT, sTr=sTr, tt=tt):
                            qt, qr = pq.next()
                            for h in range(2):
                                P.op("pe", lambda e, qt=qt, ot=ot, h=h: e.transpose(
                                    out=qt[:, h * 128:(h + 1) * 128], in_=ot[:, h * 128:(h + 1) * 128], identity=identb[:]),
                                    r=[orr, r_identb], w=[qr], inc=(h == 1))
                            P.op("act", lambda e, qt=qt, sT=sT, tt=tt: e.copy(
                                out=sT[:, :, tt * 128:(tt + 1) * 128], in_=qt[:].rearrange("p (h t) -> p h t", h=2)),
                                r=[qr], w=[sTr])
                        post[0] = pst
                    elif kind == "va":
                        P.op("act", lambda e, ppt=ppt, sV=sV, tt=tt: e.copy(out=sV[:, tt, :], in_=ppt[:]),
                             r=[ppr], w=[sVr])
                    elif kind == "kbvb":
                        obf.next()
                        rope(ppt, ppr, gt, 1, 64, 0, 0, (0,))
                        ot, orr = obf.cur()
                        qt, qr = pq.next()
                        P.op("pe", lambda e, qt=qt, ot=ot: e.transpose(
                            out=qt[:, 0:128], in_=ot[:, 0:128], identity=identb[:]), r=[orr, r_identb], w=[qr])
                        P.op("act", lambda e, qt=qt, sT=sT, tt=tt: e.copy(
                            out=sT[:, 0, tt * 128:(tt + 1) * 128], in_=qt[:, 0:128]), r=[qr], w=[sTr])
                        P.op("act", lambda e, ppt=ppt, sV=sV, tt=tt: e.copy(out=sV[:, tt, 0:128], in_=ppt[:, 128:256]),
                             r=[ppr], w=[sVr])
                    elif kind == "qi":
                        obf.next()
                        rope(ppt, ppr, gt, 4, 32, 64, 0, (0,))
                        ot, orr = obf.cur()

                        def pst(ot=ot, orr=orr, sT=sT, sTr=sTr, tt=tt):
                            qt, qr = pq.next()
                            for h in range(2):
                                P.op("pe", lambda e, qt=qt, ot=ot, h=h: e.transpose(
                                    out=qt[:, h * 128:(h + 1) * 128], in_=ot[:, h * 128:(h + 1) * 128], identity=identb[:]),
                                    r=[orr, r_identb], w=[qr], inc=(h == 1))
                            P.op("act", lambda e, qt=qt, sT=sT, tt=tt: e.copy(
                                out=sT[:, :, tt * 128:(tt + 1) * 128], in_=qt[:].rearrange("p (h t) -> p h t", h=2)),
                                r=[qr], w=[sTr])
                        post[0] = pst
                    elif kind == "kiwi":
                        obf.next()
                        rope(ppt, ppr, gt, 1, 32, 64, 0, (0,))
                        rope(ppt, ppr, gt, 1, 32, 64, 64, (0,))
                        ot, orr = obf.cur()
                        qt, qr = pq.next()
                        P.op("pe", lambda e, qt=qt, ot=ot: e.transpose(
                            out=qt[:, 0:128], in_=ot[:, 0:128], identity=identb[:]), r=[orr, r_identb], w=[qr])
                        P.op("act", lambda e, qt=qt, sT=sT, tt=tt: e.copy(
                            out=sT[:, 0, tt * 128:(tt + 1) * 128], in_=qt[:, 0:128]), r=[qr], w=[sTr])
                        if own:
                            P.op("act", lambda e, ppt=ppt, tt=tt: e.copy(out=wist[:, tt, :], in_=ppt[:, 64:80]),
                                 r=[ppr], w=[r_wist])
                if post[0] is not None:
                    post[0]()
                    post[0] = None
                if kind in ("qa", "qb"):
                    h0 = (c0 // 128) if kind == "qa" else 16 + (c0 - 3072) // 128
                    P.dma("sp", qT_d[h0:h0 + 2, :, s * 512:(s + 1) * 512].rearrange("h p t -> p h t"), sT[:, :, s * 512:(s + 1) * 512], r=[sTr])
                elif kind == "ka":
                    h0 = (c0 - 2048) // 128
                    for h in range(2):
                        dst = kTa_d[h0 + h].rearrange("p (tt gg i) -> p tt gg i", tt=NT, gg=4)[:, s * 4:(s + 1) * 4, g, :]
                        P.dma("sp", dst, sT[:, h, :].rearrange("p (tt i) -> p tt i", tt=NT)[:, s * 4:(s + 1) * 4, :], r=[sTr])
                elif kind == "va":
                    cc = c0 - 2560
                    dst = va_d.rearrange("(tt gg i) c -> i tt gg c", tt=NT, gg=4)[:, s * 4:(s + 1) * 4, g, cc:cc + 256]
                    P.dma("sp", dst, sV[:, s * 4:(s + 1) * 4, :], r=[sVr])
                elif kind == "kbvb":
                    dst = kTb_d.rearrange("p (tt gg i) -> p tt gg i", tt=NT, gg=4)[:, s * 4:(s + 1) * 4, g, :]
                    P.dma("sp", dst, sT[:, 0, :].rearrange("p (tt i) -> p tt i", tt=NT)[:, s * 4:(s + 1) * 4, :], r=[sTr])
                    dst = vb_d.rearrange("(tt gg i) c -> i tt gg c", tt=NT, gg=4)[:, s * 4:(s + 1) * 4, g, :]
                    P.dma("sp", dst, sV[:, s * 4:(s + 1) * 4, 0:128], r=[sVr])
                elif kind == "qi":
                    h0 = (c0 - 5376) // 128
                    P.dma("sp", qiT_d[h0:h0 + 2, :, s * 512:(s + 1) * 512].rearrange("h p t -> p h t"), sT[:, :, s * 512:(s + 1) * 512], r=[sTr])
                elif kind == "kiwi":
                    dst = kiT_d.rearrange("p (tt gg i) -> p tt gg i", tt=NT, gg=4)[:, s * 4:(s + 1) * 4, g, :]
                    P.dma("sp", dst, sT[:, 0, :].rearrange("p (tt i) -> p tt i", tt=NT)[:, s * 4:(s + 1) * 4, :], r=[sTr])
                    if own:
                        P.dma("sp", wi_d.rearrange("(tt i) c -> i tt c", tt=NT)[:, s * 4:(s + 1) * 4, :], wist[:, s * 4:(s + 1) * 4, :], r=[r_wist])

        units = [(g, s) for g in (1, 2, 3, 0) for s in range(2)]
        norm(*units[0])
        for k_, (g, s) in enumerate(units):
            trans(g, s)
            if k_ + 1 < len(units):
                norm(*units[k_ + 1])
            mm(g, s)
        P.barrier()


def rope_inv():
    inv128 = np.power(np.float32(10000.0), -np.arange(0, 128, 2, dtype=np.float32) / np.float32(128)).astype(np.float32)
    inv64 = np.power(np.float32(10000.0), -np.arange(0, 64, 2, dtype=np.float32) / np.float32(64)).astype(np.float32)
    return np.concatenate([inv128, inv64]).astype(np.float32)


def col_layout(v):
    return np.ascontiguousarray(np.asarray(v, dtype=np.float32).reshape(KC, 128).T)


def prep_shared(inputs, stage):
    sh = {
        "ln1_col": col_layout(inputs["ln1_g"][0]),
        "ln2_col": col_layout(inputs["ln2_g"][0]),
        "lnf_row": np.ascontiguousarray(inputs["lnf_g"], dtype=np.float32),
        "b_ada": np.ascontiguousarray(inputs["b_ada"][0], dtype=np.float32),
        "w_ada": np.ascontiguousarray(inputs["w_ada"][0], dtype=np.float32),
        "w_in": np.ascontiguousarray(inputs["w_in"][0], dtype=np.float32),
        "w_out": np.ascontiguousarray(inputs["w_out"][0], dtype=np.float32),
        "wq": np.ascontiguousarray(inputs["peer_wq"][0], dtype=np.float32),
        "subkT": np.ascontiguousarray(np.asarray(inputs["peer_sub_keys"][0], dtype=np.float32)
                                      .reshape(16, 128, 128).transpose(2, 0, 1)),
        "ident": np.eye(128, dtype=np.float32),
        "inv_bc": np.ascontiguousarray(np.broadcast_to(rope_inv()[None, :], (128, 96))),
        "iota128": np.ascontiguousarray(np.broadcast_to(np.arange(128, dtype=np.float32)[None, :], (128, 128))),
    }
    if stage >= 4:
        sh["uT"] = np.ascontiguousarray(np.asarray(inputs["peer_u"][0], dtype=np.float32).T)
        sh["vtab"] = np.ascontiguousarray(inputs["peer_v"][0], dtype=np.float32)
    return sh


def prep_core(inputs, shared, b, q):
    x = np.asarray(inputs["x"], dtype=np.float32)
    pos = np.asarray(inputs["positions"], dtype=np.int32)
    xg = np.stack([x[b, ((q + g) % 4)::4, :] for g in range(4)], axis=0)
    pg = np.stack([pos[b, ((q + g) % 4)::4] for g in range(4)], axis=0)
    posT = np.ascontiguousarray(pg.reshape(4, NT, 128).transpose(2, 0, 1).reshape(128, 32))
    m = dict(shared)
    m["xg"] = np.ascontiguousarray(xg)
    m["posT"] = posT.astype(np.int32)
    m["c_col"] = col_layout(np.asarray(inputs["c"], dtype=np.float32)[b])
    mA, cB = make_masks(q)
    m["maskA"] = mA
    m["cmaskB"] = cB
    return m


def sm_front(P, W, S_sb, S_r, Pt, P_r, st):
    scale = 128 ** -0.5
    stt, r_st = st
    P.op("dve", lambda e: e.tensor_reduce(out=stt[:, 0:1], in_=S_sb[:, 0:W], axis=AX.X, op=ALU.max),
         r=[S_r], w=[r_st])
    P.op("dve", lambda e: e.tensor_scalar(out=stt[:, 1:2], in0=stt[:, 0:1], scalar1=-scale, scalar2=None, op0=ALU.mult),
         r=[r_st], w=[r_st])
    P.op("act", lambda e: e.activation(out=Pt[:, 0:W], in_=S_sb[:, 0:W], func=AF.Exp, scale=scale,
                                       bias=stt[:, 1:2], accum_out=stt[:, 2:3]),
         r=[S_r, r_st], w=[P_r, r_st])


def sm_back(P, W, Pt, P_r, PTt, PT_r, PTps, Ops, vtile, oTst, oT_r, hslot, st, r_identb, identb):
    stt, r_st = st
    P.op("dve", lambda e: e.reciprocal(out=stt[:, 3:4], in_=stt[:, 2:3]), r=[r_st], w=[r_st])
    P.op("dve", lambda e: e.tensor_scalar(out=Pt[:, 0:W], in0=Pt[:, 0:W], scalar1=stt[:, 3:4], scalar2=None, op0=ALU.mult),
         r=[P_r, r_st], w=[P_r])
    ntile = W // 128
    for c0 in range(0, ntile, 8):
        c1 = min(ntile, c0 + 8)
        pt, pr = PTps.next()
        for k in range(c0, c1):
            P.op("pe", lambda e, pt=pt, k=k, c0=c0: e.transpose(
                out=pt[:, (k - c0) * 128:(k - c0 + 1) * 128], in_=Pt[:, k * 128:(k + 1) * 128], identity=identb[:]),
                r=[P_r, r_identb], w=[pr], inc=(k == c1 - 1))
        P.op("act", lambda e, pt=pt, c0=c0, c1=c1: e.copy(out=PTt[:, c0 * 128:c1 * 128], in_=pt[:, 0:(c1 - c0) * 128]),
             r=[pr], w=[PT_r])
    ot, orr = Ops.next()
    for k in range(ntile):
        vt, vr = vtile(k)
        P.op("pe", lambda e, ot=ot, vt=vt, k=k: e.matmul(
            ot[:, 0:128], lhsT=vt, rhs=PTt[:, k * 128:(k + 1) * 128], start=(k == 0), stop=(k == ntile - 1)),
            r=[PT_r, vr], w=[orr], inc=(k == ntile - 1))
    P.op("act", lambda e, ot=ot: e.copy(out=oTst[:, hslot, :], in_=ot[:, 0:128]), r=[orr], w=[oT_r])


def phase2a(nc, P, L):
    identb, r_identb = L["identb"], L["r_identb"]
    qT_d, kTa_d, va_d, oT_d, maskA_in = (L[k] for k in ("qT_d", "kTa_d", "va_d", "oT_d", "maskA_in"))
    with ExitStack() as ph:
        def sbp(name, shape, dt):
            return ph.enter_context(nc.sbuf_tensor(name, shape, dt))

        def psp(name, shape, dt):
            return ph.enter_context(nc.psum_tensor(name, shape, dt))

        KT = sbp("KTa", [128, 4, 4096], BF16)
        VA = sbp("VAr", [128, 32, 512], BF16)
        mA = sbp("mA", [128, 15, 128], F32)
        r_KT, r_VA, r_mA = P.res("KTa"), P.res("VAr"), P.res("mA")
        for h in range(4):
            P.dma("sp", KT[:, h, :], kTa_d[h], w=[r_KT])
        P.dma("sp", VA[:], va_d.rearrange("(tl p) c -> p tl c", p=128), w=[r_VA])
        P.dma("sp", mA[:], maskA_in, w=[r_mA])
        qblk = Buf(P, sbp, "qblkA", [128, 16, 128], BF16, 2)
        Ssb = Buf(P, sbp, "SsbA", [128, 1920], F32, 2)
        Pb = Buf(P, sbp, "PbA", [128, 1920], BF16, 2)
        PTb = Buf(P, sbp, "PTbA", [128, 1920], BF16, 2)
        oTst = Buf(P, sbp, "oTstA", [128, 16, 128], BF16, 2)
        stb = Buf(P, sbp, "stA", [128, 4], F32, 2)
        Sps = Buf(P, psp, "SpsA", [128, 512], F32, 4)
        PTps = Buf(P, psp, "PTpsA", [128, 1024], BF16, 2)
        Ops = Buf(P, psp, "OpsA", [128, 512], F32, 2)
        pending = [None]
        for n in range(NT):
            qt, qr = qblk.next()
            P.dma("sp", qt[:], qT_d[0:16, :, n * 128:(n + 1) * 128].rearrange("h p t -> p h t"), w=[qr])
            ost, osr = oTst.next()
            slots = []
            if n > 0:
                slots.append(((n - 1) * 512, 512, 0))
            slots.append((n * 512, 512, 4))
            if n > 0:
                slots.append(((n - 1) * 512, 128, 8))
            slots.append((n * 512, 128, 9))
            for k in range(4, -1, -1):
                if n - k >= 0:
                    slots.append(((n - k) * 512, 128, 10 + (4 - k)))
            W = sum(sl[1] for sl in slots)
            for kv in range(4):
                for hq in range(4):
                    head = kv * 4 + hq
                    St, Sr = Ssb.next()
                    off = 0
                    bank, bw = None, 512
                    groups = []
                    for (p0, w, m0) in slots:
                        if bw + w > 512:
                            bank = Sps.next()
                            groups.append([bank, []])
                            bw = 0
                        groups[-1][1].append((bw, p0, w, m0))
                        bw += w
                    for (bt, br), items in groups:
                        for i, (bo, p0, w, m0) in enumerate(items):
                            P.op("pe", lambda e, bt=bt, bo=bo, p0=p0, w=w, kv=kv, head=head, qt=qt: e.matmul(
                                bt[:, bo:bo + w], lhsT=qt[:, head, :], rhs=KT[:, kv, p0:p0 + w], start=True, stop=True),
                                r=[qr, r_KT], w=[br], inc=(i == len(items) - 1))
                        runs = []
                        for (bo, p0, w, m0) in items:
                            if runs and runs[-1][2] + runs[-1][1] // 128 == m0:
                                runs[-1][1] += w
                            else:
                                runs.append([bo, w, m0])
                        for (bo, tw, m0) in runs:
                            P.op("dve", lambda e, bt=bt, St=St, off=off, tw=tw, m0=m0, bo=bo: e.tensor_tensor(
                                out=St[:, off:off + tw], in0=bt[:, bo:bo + tw],
                                in1=mA[:, m0:m0 + tw // 128, :].rearrange("p a b -> p (a b)"), op=ALU.add),
                                r=[br, r_mA], w=[Sr])
                            off += tw
                    vt_list = []
                    for (p0, w, m0) in slots:
                        for k in range(w // 128):
                            vt_list.append(p0 // 128 + k)
                    Pt, Pr = Pb.next()
                    PTt, PTr = PTb.next()
                    st = stb.next()
                    sm_front(P, W, St, Sr, Pt, Pr, st)

                    def back(W=W, Pt=Pt, Pr=Pr, PTt=PTt, PTr=PTr, kv=kv, vt_list=vt_list, ost=ost, osr=osr,
                             head=head, st=st, n=n):
                        sm_back(P, W, Pt, Pr, PTt, PTr, PTps, Ops,
                                lambda k: (VA[:, vt_list[k], kv * 128:(kv + 1) * 128], r_VA),
                                ost, osr, head, st, r_identb, identb)
                        if head == 15:
                            P.dma("sp", oT_d[0:16, :, n * 128:(n + 1) * 128].rearrange("h p t -> p h t"), ost[:], r=[osr])

                    if pending[0] is not None:
                        pending[0]()
                    pending[0] = back
        if pending[0] is not None:
            pending[0]()
        P.barrier()


def phase2b(nc, P, L):
    identb, r_identb = L["identb"], L["r_identb"]
    qT_d, kTb_d, vb_d, kiT_d, qiT_d, wi_d, oT_d, cmaskB_in = (L[k] for k in
        ("qT_d", "kTb_d", "vb_d", "kiT_d", "qiT_d", "wi_d", "oT_d", "cmaskB_in"))
    NIT = 26
    with ExitStack() as ph:
        def sbp(name, shape, dt):
            return ph.enter_context(nc.sbuf_tensor(name, shape, dt))

        def psp(name, shape, dt):
            return ph.enter_context(nc.psum_tensor(name, shape, dt))

        KB = sbp("KTb", [128, 4096], BF16)
        VB = sbp("VBr", [128, 32, 128], BF16)
        KI = sbp("KIr", [128, 4096], BF16)
        cm = sbp("cmB", [128, 4, 128], F32)
        r_KB, r_VB, r_KI, r_cm = P.res("KTb"), P.res("VBr"), P.res("KIr"), P.res("cmB")
        P.dma("sp", KB[:], kTb_d, w=[r_KB])
        P.dma("sp", VB[:], vb_d.rearrange("(tl p) c -> p tl c", p=128), w=[r_VB])
        P.dma("sp", KI[:], kiT_d, w=[r_KI])
        P.dma("sp", cm[:], cmaskB_in, w=[r_cm])
        qblk = Buf(P, sbp, "qblkB", [128, 16, 128], BF16, 2)
        qiblk = Buf(P, sbp, "qiblk", [128, 8, 128], BF16, 2)
        wib = Buf(P, sbp, "wib", [128, 16], F32, 2)
        scoreb = Buf(P, sbp, "score", [128, 4096], F32, 2)
        addmb = Buf(P, sbp, "addm", [128, 4096], BF16, 2)
        junkb = sbp("junkb", [128, 4096], BF16)
        r_junkb = P.res("junkb")
        relb = Buf(P, sbp, "relb", [128, 512], F32, 4)
        bstb = Buf(P, sbp, "bst", [128, 8], F32, 2)
        Ssb = Buf(P, sbp, "SsbB", [128, 4096], F32, 2)
        Pb = Buf(P, sbp, "PbB", [128, 4096], BF16, 2)
        PTb = Buf(P, sbp, "PTbB", [128, 4096], BF16, 2)
        oTst = Buf(P, sbp, "oTstB", [128, 16, 128], BF16, 2)
        stb = Buf(P, sbp, "stB", [128, 4], F32, 2)
        Rps = Buf(P, psp, "RpsB", [128, 512], F32, 2)
        Sps = Buf(P, psp, "SpsB", [128, 512], F32, 2)
        PTps = Buf(P, psp, "PTpsB", [128, 1024], BF16, 2)
        Ops = Buf(P, psp, "OpsB", [128, 512], F32, 2)

        def front(n, out):
            Kn = (n + 1) * 512
            nch = n + 1
            qt, qr = qblk.next()
            P.dma("sp", qt[:], qT_d[16:32, :, n * 128:(n + 1) * 128].rearrange("h p t -> p h t"), w=[qr])
            qit, qir = qiblk.next()
            P.dma("sp", qit[:], qiT_d[:, :, n * 128:(n + 1) * 128].rearrange("h p t -> p h t"), w=[qir])
            wt, wr = wib.next()
            P.dma("sp", wt[:], wi_d[n * 128:(n + 1) * 128, :], w=[wr])
            score, r_score = scoreb.next()
            addm, r_addm = addmb.next()
            bst, r_bst = bstb.next()
            out.update(qt=qt, qr=qr, addm=addm, r_addm=r_addm, Kn=Kn, nch=nch)
            yield
            for hp in range(8):
                for half in range(2):
                    hd = hp * 2 + half
                    ps0 = half * 64
                    for kc in range(nch):
                        rt, rr = Rps.next()
                        P.op("pe", lambda e, rt=rt, qit=qit, hp=hp, ps0=ps0, kc=kc: e.matmul(
                            rt[:], lhsT=qit[ps0:ps0 + 64, hp, :], rhs=KI[ps0:ps0 + 64, kc * 512:(kc + 1) * 512],
                            start=True, stop=True), r=[qir, r_KI], w=[rr])
                        lt, lr = relb.next()
                        P.op("act", lambda e, rt=rt, lt=lt: e.activation(out=lt[:], in_=rt[:], func=AF.Relu, scale=1.0 / 32.0),
                             r=[rr], w=[lr])
                        dst = score[:, kc * 512:(kc + 1) * 512]
                        if hd == 0:
                            P.op("act", lambda e, lt=lt, dst=dst, wt=wt, hd=hd: e.activation(
                                out=dst, in_=lt[:], func=AF.Identity, scale=wt[:, hd:hd + 1]),
                                r=[lr, wr], w=[r_score])
                        else:
                            P.op("act", lambda e, lt=lt, wt=wt, hd=hd: e.activation(
                                out=lt[:], in_=lt[:], func=AF.Identity, scale=wt[:, hd:hd + 1]),
                                r=[lr, wr], w=[lr])
                            P.op("pool", lambda e, lt=lt, dst=dst: e.tensor_tensor(out=dst, in0=dst, in1=lt[:], op=ALU.add),
                                 r=[lr, r_score], w=[r_score])
                        yield
            dg = score[:, Kn - 512:Kn]
            P.op("dve", lambda e, dg=dg: e.tensor_tensor(out=dg, in0=dg, in1=cm[:].rearrange("p a b -> p (a b)"), op=ALU.add),
                 r=[r_score, r_cm], w=[r_score])
            P.op("dve", lambda e: e.tensor_reduce(out=bst[:, 0:1], in_=score[:, 0:Kn], axis=AX.X, op=ALU.max),
                 r=[r_score], w=[r_bst])
            P.op("dve", lambda e: e.tensor_scalar(out=bst[:, 1:2], in0=bst[:, 0:1], scalar1=-64.0, scalar2=None, op0=ALU.add),
                 r=[r_bst], w=[r_bst])
            yield
            for it in range(NIT):
                ck = 64.0 / (2.0 ** (it + 1))
                P.op("dve", lambda e, ck=ck: e.tensor_scalar(out=bst[:, 2:3], in0=bst[:, 1:2], scalar1=ck, scalar2=None, op0=ALU.add),
                     r=[r_bst], w=[r_bst])
                P.op("dve", lambda e: e.tensor_scalar(out=junkb[:, 0:Kn], in0=score[:, 0:Kn], scalar1=bst[:, 2:3],
                                                      scalar2=None, op0=ALU.is_ge, op1=ALU.add, accum_out=bst[:, 3:4]),
                     r=[r_score, r_bst], w=[r_junkb, r_bst])
                yield
                P.op("dve", lambda e: e.tensor_scalar(out=bst[:, 4:5], in0=bst[:, 3:4], scalar1=255.5, scalar2=None, op0=ALU.is_gt),
                     r=[r_bst], w=[r_bst])
                P.op("dve", lambda e, ck=ck: e.scalar_tensor_tensor(out=bst[:, 1:2], in0=bst[:, 4:5], scalar=ck, in1=bst[:, 1:2],
                                                                 op0=ALU.mult, op1=ALU.add), r=[r_bst], w=[r_bst])
                yield
            P.op("dve", lambda e: e.tensor_scalar(out=addm[:, 0:Kn], in0=score[:, 0:Kn], scalar1=bst[:, 1:2], scalar2=NEG,
                                                  op0=ALU.is_lt, op1=ALU.mult), r=[r_score, r_bst], w=[r_addm])
            yield

        def nsteps(n):
            return 1 + 16 * (n + 1) + 1 + 2 * NIT + 1

        cur = {}
        pending = [None]
        for _ in front(0, cur):
            pass
        for n in range(NT):
            nxt = {}
            genbox = [front(n + 1, nxt) if n + 1 < NT else None]
            per_head = (nsteps(n + 1) + 15) // 16 if genbox[0] is not None else 0

            def adv(k, genbox=genbox):
                for _ in range(k):
                    if genbox[0] is None:
                        return
                    try:
                        next(genbox[0])
                    except StopIteration:
                        genbox[0] = None
                        return
            qt, qr, addm, r_addm, Kn, nch = (cur[k] for k in ("qt", "qr", "addm", "r_addm", "Kn", "nch"))
            ost, osr = oTst.next()
            for h in range(16):
                St, Sr = Ssb.next()
                for kc in range(nch):
                    bt, br = Sps.next()
                    P.op("pe", lambda e, bt=bt, qt=qt, h=h, kc=kc: e.matmul(
                        bt[:], lhsT=qt[:, h, :], rhs=KB[:, kc * 512:(kc + 1) * 512], start=True, stop=True),
                        r=[qr, r_KB], w=[br])
                    P.op("dve", lambda e, bt=bt, St=St, kc=kc, addm=addm: e.tensor_tensor(
                        out=St[:, kc * 512:(kc + 1) * 512], in0=bt[:], in1=addm[:, kc * 512:(kc + 1) * 512], op=ALU.add),
                        r=[br, r_addm], w=[Sr])
                    adv(1)
                Pt, Pr = Pb.next()
                PTt, PTr = PTb.next()
                st = stb.next()
                sm_front(P, Kn, St, Sr, Pt, Pr, st)
                rem = max(0, per_head - nch)
                adv(rem // 2)

                def back(Kn=Kn, Pt=Pt, Pr=Pr, PTt=PTt, PTr=PTr, ost=ost, osr=osr, h=h, st=st, n=n):
                    sm_back(P, Kn, Pt, Pr, PTt, PTr, PTps, Ops, lambda k: (VB[:, k, :], r_VB),
                            ost, osr, h, st, r_identb, identb)
                    if h == 15:
                        P.dma("sp", oT_d[16:32, :, n * 128:(n + 1) * 128].rearrange("h p t -> p h t"), ost[:], r=[osr])

                if pending[0] is not None:
                    pending[0]()
                pending[0] = back
                adv(rem - rem // 2)
            if genbox[0] is not None:
                for _ in genbox[0]:
                    pass
                genbox[0] = None
            cur = nxt
        if pending[0] is not None:
            pending[0]()
        P.barrier()


def phase2c(nc, P, L):
    xg, w_out, oT_d, x1_d, mod_d = (L[k] for k in ("xg", "w_out", "oT_d", "x1_d", "mod_d"))
    with ExitStack() as ph:
        def sbp(name, shape, dt):
            return ph.enter_context(nc.sbuf_tensor(name, shape, dt))

        def psp(name, shape, dt):
            return ph.enter_context(nc.psum_tensor(name, shape, dt))

        oT = sbp("oTall", [128, 32, T], BF16)
        r_oT = P.res("oTall")
        P.dma("sp", oT[:], oT_d.rearrange("h p t -> p h t"), w=[r_oT])
        g1 = sbp("g1bc", [128, D], F32)
        r_g1 = P.res("g1bc")
        P.dma("sp", g1[:], mod_d[2 * D:3 * D].partition_broadcast(128), w=[r_g1])
        wb = Buf(P, sbp, "woutb", [128, 32, 512], BF16, 2)
        xb = Buf(P, sbp, "xres", [128, 512], F32, 3)
        yb = Buf(P, sbp, "ytmp", [128, 512], F32, 3)
        yps = Buf(P, psp, "yps", [128, 512], F32, 4)
        w_out_v = w_out.rearrange("(h p) n -> p h n", p=128)
        for nc_ in range(8):
            wt, wr = wb.next()
            P.dma("pool", wt[:], w_out_v[:, :, nc_ * 512:(nc_ + 1) * 512], w=[wr])
            for tt in range(NT):
                xt, xr = xb.next()
                P.dma("sp", xt[:], xg[0, tt * 128:(tt + 1) * 128, nc_ * 512:(nc_ + 1) * 512], w=[xr])
                pt, pr = yps.next()
                for h in range(32):
                    P.op("pe", lambda e, pt=pt, wt=wt, h=h, tt=tt: e.matmul(
                        pt[:], lhsT=oT[:, h, tt * 128:(tt + 1) * 128], rhs=wt[:, h, :], start=(h == 0), stop=(h == 31)),
                        r=[r_oT, wr], w=[pr], inc=(h == 31))
                yt, yr = yb.next()
                P.op("dve", lambda e, pt=pt, yt=yt, nc_=nc_: e.tensor_tensor(
                    out=yt[:], in0=pt[:], in1=g1[:, nc_ * 512:(nc_ + 1) * 512], op=ALU.mult), r=[pr, r_g1], w=[yr])
                P.op("pool", lambda e, yt=yt, xt=xt: e.tensor_tensor(out=yt[:], in0=yt[:], in1=xt[:], op=ALU.add),
                     r=[yr, xr], w=[yr])
                P.dma("sp", x1_d[tt * 128:(tt + 1) * 128, nc_ * 512:(nc_ + 1) * 512], yt[:], r=[yr])
        P.barrier()


def make_masks(q):
    p = np.arange(128)[:, None]
    j = np.arange(128)[None, :]
    mA = np.zeros((128, 15, 128), dtype=np.float32)

    def put(idx, valid):
        mA[:, idx, :] = np.where(valid, 0.0, NEG).astype(np.float32)

    for g in range(4):
        dg = q - ((q + g) % 4)
        d1 = 4 * (128 + p - j) + dg
        put(g, (d1 >= 0) & (d1 <= 128))
        d0 = 4 * (p - j) + dg
        put(4 + g, (d0 >= 0) & (d0 <= 128))
    put(8, j >= p)
    put(9, j <= p)
    for k in range(4, -1, -1):
        df = 128 * k + p - j
        put(10 + (4 - k), (df % 4 == 0) & (df >= 0) & (df <= 512))
    cB = np.zeros((128, 4, 128), dtype=np.float32)
    for g in range(4):
        rg = (q + g) % 4
        cB[:, g, :] = np.where((j < p) | ((j == p) & (rg <= q)), 0.0, NEG)
    return mA, cB


def phase3(nc, P, L):
    x1_d, wq, subkT, h2T_d, GT_d, iota_in = (L[k] for k in ("x1_d", "wq", "subkT", "h2T_d", "GT_d", "iota_in"))
    identb, r_identb, identf, r_identf = L["identb"], L["r_identb"], L["identf"], L["r_identf"]
    A2, B2, r_A2, r_modcol = L["A2"], L["B2"], L["r_A2"], L["r_modcol"]
    with ExitStack() as ph3:
        qpT = ph3.enter_context(nc.sbuf_tensor("qpT", [128, 16, T], F32))
        r_qp = [P.res("qpT%d" % i) for i in range(2)]
        with ExitStack() as ph:
            def sbp(name, shape, dt):
                return ph.enter_context(nc.sbuf_tensor(name, shape, dt))

            def psp(name, shape, dt):
                return ph.enter_context(nc.psum_tensor(name, shape, dt))

            hT = sbp("h2T", [128, KC, T], BF16)
            r_hT = [P.res("h2T%d" % i) for i in range(2)]
            xf = Buf(P, sbp, "x1f", [128, D], F32, 1)
            xs4 = sbp("x1s4", [128, 4, D], BF16)
            r_xs = [P.res("x1s%d" % j) for j in range(4)]
            stat = sbp("stat3", [128, 8], F32)
            r_stat = P.res("stat3")
            wqb = Buf(P, sbp, "wqb", [128, KC, 128], BF16, 2)
            pT = Buf(P, psp, "pT3", [128, 512], BF16, 2)
            qps = Buf(P, psp, "qps", [128, 512], F32, 2)
            ei = 0
            for s_ in range(2):
                for j in range(4):
                    tt = s_ * 4 + j
                    xt, xr = xf.next()
                    P.dma("sp", xt[:], x1_d[tt * 128:(tt + 1) * 128, :], w=[xr])
                    P.op("act", lambda e, xt=xt, j=j: e.activation(out=xs4[:, j, :], in_=xt[:], func=AF.Square,
                                                                accum_out=stat[:, j:j + 1]), r=[xr], w=[r_xs[j], r_stat])
                    P.op("act", lambda e, j=j: e.activation(out=stat[:, j:j + 1], in_=stat[:, j:j + 1], func=AF.Sqrt,
                                                         scale=1.0 / D, bias=EPS), r=[r_stat], w=[r_stat])
                    P.op("dve", lambda e, j=j: e.reciprocal(out=stat[:, 4 + j:5 + j], in_=stat[:, j:j + 1]),
                         r=[r_stat], w=[r_stat])
                    P.op("dve", lambda e, xt=xt, j=j: e.tensor_scalar(out=xs4[:, j, :], in0=xt[:], scalar1=stat[:, 4 + j:5 + j],
                                                                   scalar2=None, op0=ALU.mult), r=[xr, r_stat], w=[r_xs[j]])
                for kc in range(KC):
                    pt, pr = pT.next()
                    for j in range(4):
                        P.op("pe", lambda e, pt=pt, j=j, kc=kc: e.transpose(
                            out=pt[:, j * 128:(j + 1) * 128], in_=xs4[:, j, kc * 128:(kc + 1) * 128], identity=identb[:]),
                            r=[r_xs[j], r_identb], w=[pr], inc=(j == 3))
                    dst = hT[:, kc, s_ * 512:(s_ + 1) * 512]
                    if ei % 2 == 0:
                        P.op("dve", lambda e, pt=pt, dst=dst, kc=kc: e.tensor_scalar(
                            out=dst, in0=pt[:], scalar1=A2[:, kc:kc + 1], scalar2=B2[:, kc:kc + 1],
                            op0=ALU.mult, op1=ALU.add), r=[pr, r_A2, r_modcol], w=[r_hT[s_]])
                    else:
                        P.op("act", lambda e, pt=pt, dst=dst, kc=kc: e.activation(
                            out=dst, in_=pt[:], func=AF.Identity, scale=A2[:, kc:kc + 1], bias=B2[:, kc:kc + 1]),
                            r=[pr, r_A2, r_modcol], w=[r_hT[s_]])
                    ei += 1
            P.dma("sp", h2T_d[:, :, 0:512], hT[:, :, 0:512], r=[r_hT[0]])
            P.dma("sp", h2T_d[:, :, 512:1024], hT[:, :, 512:1024], r=[r_hT[1]])
            wq_v = wq.rearrange("(kc p) n -> p kc n", p=128)
            for cc in range(16):
                wt, wr = wqb.next()
                P.dma("pool", wt[:], wq_v[:, :, cc * 128:(cc + 1) * 128], w=[wr])
                for hf in range(2):
                    qt, qr = qps.next()
                    for kc in range(KC):
                        P.op("pe", lambda e, qt=qt, wt=wt, kc=kc, hf=hf: e.matmul(
                            qt[:], lhsT=wt[:, kc, :], rhs=hT[:, kc, hf * 512:(hf + 1) * 512],
                            start=(kc == 0), stop=(kc == KC - 1)), r=[wr, r_hT[hf]], w=[qr], inc=(kc == KC - 1))
                    P.op("act", lambda e, qt=qt, cc=cc, hf=hf: e.copy(out=qpT[:, cc, hf * 512:(hf + 1) * 512], in_=qt[:]),
                         r=[qr], w=[r_qp[hf]])
            P.barrier()
        with ExitStack() as ph:
            def sbp(name, shape, dt):
                return ph.enter_context(nc.sbuf_tensor(name, shape, dt))

            def psp(name, shape, dt):
                return ph.enter_context(nc.psum_tensor(name, shape, dt))

            skT = sbp("skT", [128, 16, 128], F32)
            iot = sbp("iot", [128, 128], F32)
            r_sk, r_iot = P.res("skT"), P.res("iot")
            P.dma("sp", skT[:], subkT, w=[r_sk])
            P.dma("sp", iot[:], iota_in, w=[r_iot])
            ssb = sbp("ssb", [128, 2048], F32)
            s2 = sbp("s2", [128, 2048], F32)
            cand = sbp("cand", [128, 8, 256], F32)
            cand2 = sbp("cand2", [128, 8, 256], F32)
            oh = sbp("oh", [128, 8, 256], F32)
            v16 = sbp("v16", [128, 16, 16], F32)
            i16 = sbp("i16", [128, 16, 16], U32)
            i16f = sbp("i16f", [128, 16, 16], F32)
            c16 = sbp("c16", [128, 8, 16], F32)
            p16 = sbp("p16", [128, 8, 16], U32)
            pa = sbp("pa", [128, 8, 16], U32)
            pb = sbp("pb", [128, 8, 16], U32)
            paf = sbp("paf", [128, 8, 16], F32)
            pbf = sbp("pbf", [128, 8, 16], F32)
            g16 = sbp("g16", [128, 8, 16], F32)
            e16 = sbp("e16", [128, 8, 16], F32)
            z8 = sbp("z8", [128, 8], F32)
            i0f = sbp("i0f", [128, 128], F32)
            i1f = sbp("i1f", [128, 128], F32)
            trT = sbp("trT", [128, 3, 128], F32)
            Bm = sbp("Bm", [128, 64, 128], BF16)
            gA = sbp("gA", [128, 64, 128], BF16)
            Gst = Buf(P, sbp, "Gst", [128, 128, 64], BF16, 2)
            r = {k: P.res(k) for k in ("ssb", "s2", "cand", "cand2", "oh", "v16", "i16", "c16", "p16", "pab", "g16",
                                       "i01", "trT", "Bm", "gA")}
            sps = Buf(P, psp, "sps", [128, 512], F32, 4)
            trps = Buf(P, psp, "trps", [128, 512], F32, 1)
            Gps = Buf(P, psp, "Gps", [128, 512], F32, 3)
            v16v = v16[:].rearrange("p (h two) k -> p h two k", two=2)
            i16v = i16f[:].rearrange("p (h two) k -> p h two k", two=2)
            for tt in range(NT):
                for qd in range(4):
                    st_, sr_ = sps.next()
                    for k in range(4):
                        hp = qd * 4 + k
                        P.op("pe", lambda e, st_=st_, k=k, hp=hp, tt=tt: e.matmul(
                            st_[:, k * 128:(k + 1) * 128], lhsT=qpT[:, hp, tt * 128:(tt + 1) * 128], rhs=skT[:, hp, :],
                            start=True, stop=True), r=[r_qp[tt // 4], r_sk], w=[sr_], inc=(k == 3))
                    P.op("act", lambda e, st_=st_, qd=qd: e.copy(out=ssb[:, qd * 512:(qd + 1) * 512], in_=st_[:]),
                         r=[sr_], w=[r["ssb"]])
                for hp in range(16):
                    sl = slice(hp * 128, (hp + 1) * 128)
                    P.op("dve", lambda e, hp=hp, sl=sl: e.max(out=v16[:, hp, 0:8], in_=ssb[:, sl]), r=[r["ssb"]], w=[r["v16"]])
                    P.op("dve", lambda e, hp=hp, sl=sl: e.max_index(out=i16[:, hp, 0:8], in_max=v16[:, hp, 0:8], in_values=ssb[:, sl]),
                         r=[r["ssb"], r["v16"]], w=[r["i16"]])
                    P.op("dve", lambda e, hp=hp, sl=sl: e.match_replace(out=s2[:, sl], in_to_replace=v16[:, hp, 0:8],
                                                                       in_values=ssb[:, sl], imm_value=NEG),
                         r=[r["ssb"], r["v16"]], w=[r["s2"]])
                    P.op("dve", lambda e, hp=hp, sl=sl: e.max(out=v16[:, hp, 8:16], in_=s2[:, sl]), r=[r["s2"]], w=[r["v16"]])
                    P.op("dve", lambda e, hp=hp, sl=sl: e.max_index(out=i16[:, hp, 8:16], in_max=v16[:, hp, 8:16], in_values=s2[:, sl]),
                         r=[r["s2"], r["v16"]], w=[r["i16"]])
                P.op("dve", lambda e: e.tensor_copy(out=i16f[:], in_=i16[:]), r=[r["i16"]], w=[r["i16"]])
                candv = cand[:].rearrange("p h (a b) -> p h a b", a=16)
                P.op("dve", lambda e: e.tensor_tensor(
                    out=cand[:].rearrange("p h (a b) -> p h a b", a=16)[:, 0:4],
                    in0=v16v[:, 0:4, 0, :].unsqueeze(3).to_broadcast([128, 4, 16, 16]),
                    in1=v16v[:, 0:4, 1, :].unsqueeze(2).to_broadcast([128, 4, 16, 16]), op=ALU.add),
                    r=[r["v16"]], w=[r["cand"]]) if False else None
                for h in range(8):
                    P.op("dve", lambda e, h=h: e.tensor_tensor(
                        out=cand[:, h, :].rearrange("p (a b) -> p a b", a=16),
                        in0=v16[:, 2 * h, :].unsqueeze(2).to_broadcast([128, 16, 16]),
                        in1=v16[:, 2 * h + 1, :].unsqueeze(1).to_broadcast([128, 16, 16]), op=ALU.add),
                        r=[r["v16"]], w=[r["cand"]])
                for h in range(8):
                    P.op("dve", lambda e, h=h: e.max(out=c16[:, h, 0:8], in_=cand[:, h, :]), r=[r["cand"]], w=[r["c16"]])
                    P.op("dve", lambda e, h=h: e.max_index(out=p16[:, h, 0:8], in_max=c16[:, h, 0:8], in_values=cand[:, h, :]),
                         r=[r["cand"], r["c16"]], w=[r["p16"]])
                    P.op("dve", lambda e, h=h: e.match_replace(out=cand2[:, h, :], in_to_replace=c16[:, h, 0:8],
                                                              in_values=cand[:, h, :], imm_value=NEG),
                         r=[r["cand"], r["c16"]], w=[r["cand2"]])
                    P.op("dve", lambda e, h=h: e.max(out=c16[:, h, 8:16], in_=cand2[:, h, :]), r=[r["cand2"]], w=[r["c16"]])
                    P.op("dve", lambda e, h=h: e.max_index(out=p16[:, h, 8:16], in_max=c16[:, h, 8:16], in_values=cand2[:, h, :]),
                         r=[r["cand2"], r["c16"]], w=[r["p16"]])
                P.op("dve", lambda e: e.tensor_tensor(out=e16[:], in0=c16[:], in1=c16[:, :, 0:1].to_broadcast([128, 8, 16]),
                                                      op=ALU.subtract), r=[r["c16"]], w=[r["g16"]])
                P.op("act", lambda e: e.activation(out=e16[:], in_=e16[:], func=AF.Exp), r=[r["g16"]], w=[r["g16"]])
                P.op("dve", lambda e: e.tensor_reduce(out=z8[:], in_=e16[:], axis=AX.X, op=ALU.add), r=[r["g16"]], w=[r["g16"]])
                P.op("dve", lambda e: e.reciprocal(out=z8[:], in_=z8[:]), r=[r["g16"]], w=[r["g16"]])
                P.op("dve", lambda e: e.tensor_tensor(out=g16[:], in0=e16[:], in1=z8[:].unsqueeze(2).to_broadcast([128, 8, 16]),
                                                      op=ALU.mult), r=[r["g16"]], w=[r["g16"]])
                P.op("dve", lambda e: e.tensor_single_scalar(out=pa[:], in_=p16[:], scalar=4, op=ALU.logical_shift_right),
                     r=[r["p16"]], w=[r["pab"]])
                P.op("dve", lambda e: e.tensor_single_scalar(out=pb[:], in_=p16[:], scalar=15, op=ALU.bitwise_and),
                     r=[r["p16"]], w=[r["pab"]])
                P.op("dve", lambda e: e.tensor_copy(out=paf[:], in_=pa[:]), r=[r["pab"]], w=[r["pab"]])
                P.op("dve", lambda e: e.tensor_copy(out=pbf[:], in_=pb[:]), r=[r["pab"]], w=[r["pab"]])
                for which, (pf, dstt) in enumerate(((paf, i0f), (pbf, i1f))):
                    for h in range(8):
                        ohv = oh[:, h, :].rearrange("p (k a) -> p k a", k=16)
                        P.op("dve", lambda e, h=h, pf=pf, ohv=ohv: e.tensor_tensor(
                            out=ohv, in0=pf[:, h, :].unsqueeze(2).to_broadcast([128, 16, 16]),
                            in1=iot[:, 0:16].unsqueeze(1).to_broadcast([128, 16, 16]), op=ALU.is_equal),
                            r=[r["pab"], r_iot], w=[r["oh"]])
                        P.op("dve", lambda e, h=h, which=which, ohv=ohv: e.tensor_tensor(
                            out=ohv, in0=ohv, in1=i16f[:, 2 * h + which, :].unsqueeze(1).to_broadcast([128, 16, 16]),
                            op=ALU.mult), r=[r["oh"], r["i16"]], w=[r["oh"]])
                    P.op("dve", lambda e, dstt=dstt: e.tensor_reduce(
                        out=dstt[:], in_=oh[:].rearrange("p h (k a) -> p (h k) a", k=16), axis=AX.X, op=ALU.add),
                        r=[r["oh"]], w=[r["i01"]])
                tp, tpr = trps.next()
                for w_, src in enumerate((i0f[:], i1f[:], g16[:].rearrange("p h k -> p (h k)"))):
                    P.op("pe", lambda e, tp=tp, w_=w_, src=src: e.transpose(
                        out=tp[:, w_ * 128:(w_ + 1) * 128], in_=src, identity=identf[:]),
                        r=[r["i01"], r["g16"], r_identf], w=[tpr], inc=(w_ == 2))
                P.op("act", lambda e, tp=tp: e.copy(out=trT[:].rearrange("p a t -> p (a t)"), in_=tp[:, 0:384]),
                     r=[tpr], w=[r["trT"]])
                for hf in range(2):
                    tsl = slice(hf * 64, (hf + 1) * 64)
                    iob = iot[:].unsqueeze(1).to_broadcast([128, 64, 128])
                    P.op("dve", lambda e, tsl=tsl, iob=iob: e.tensor_tensor(
                        out=Bm[:], in0=iob, in1=trT[:, 1, tsl].unsqueeze(2).to_broadcast([128, 64, 128]), op=ALU.is_equal),
                        r=[r_iot, r["trT"]], w=[r["Bm"]])
                    P.op("dve", lambda e, tsl=tsl, iob=iob: e.tensor_tensor(
                        out=gA[:], in0=iob, in1=trT[:, 0, tsl].unsqueeze(2).to_broadcast([128, 64, 128]), op=ALU.is_equal),
                        r=[r_iot, r["trT"]], w=[r["gA"]])
                    P.op("dve", lambda e, tsl=tsl: e.tensor_tensor(
                        out=gA[:], in0=gA[:], in1=trT[:, 2, tsl].unsqueeze(2).to_broadcast([128, 64, 128]), op=ALU.mult),
                        r=[r["gA"], r["trT"]], w=[r["gA"]])
                    gs, gsr = Gst.next()
                    for t4 in range(16):
                        gp, gpr = Gps.next()
                        for k in range(4):
                            tl = t4 * 4 + k
                            P.op("pe", lambda e, gp=gp, k=k, tl=tl: e.matmul(
                                gp[:, k * 128:(k + 1) * 128], lhsT=Bm[:, tl, :], rhs=gA[:, tl, :], start=True, stop=True),
                                r=[r["Bm"], r["gA"]], w=[gpr], inc=(k == 3))
                        src = gp[:].rearrange("p (t i) -> p i t", t=4)
                        dst = gs[:, :, t4 * 4:(t4 + 1) * 4]
                        if t4 % 2 == 0:
                            P.op("act", lambda e, src=src, dst=dst: e.copy(out=dst, in_=src), r=[gpr], w=[gsr])
                        else:
                            P.op("dve", lambda e, src=src, dst=dst: e.tensor_copy(out=dst, in_=src), r=[gpr], w=[gsr])
                    t0 = tt * 128 + hf * 64
                    for i8 in range(8):
                        P.dma("sp", GT_d[i8 * 16:(i8 + 1) * 16, :, t0:t0 + 64].rearrange("i j t -> j i t"),
                              gs[:, i8 * 16:(i8 + 1) * 16, :], r=[gsr])
            P.barrier()


def phase4(nc, P, L):
    uT, vtab, h2T_d, GT_d, actg_d, y_d, x1_d, mod_d, lnf_row, out_d = (L[k] for k in
        ("uT", "vtab", "h2T_d", "GT_d", "actg_d", "y_d", "x1_d", "mod_d", "lnf_row", "out_d"))
    NP = 64
    with ExitStack() as ph4:
        ssq = ph4.enter_context(nc.sbuf_tensor("ssq4", [128, 2, NT], F32))
        r_ssq = P.res("ssq4")
        for half in range(2):
            with ExitStack() as ph:
                def sbp(name, shape, dt):
                    return ph.enter_context(nc.sbuf_tensor(name, shape, dt))

                def psp(name, shape, dt):
                    return ph.enter_context(nc.psum_tensor(name, shape, dt))

                acc = sbp("acc%d" % half, [128, NT, 2048], F32)
                r_acc = [[P.res("acc_%d_%d" % (tt, ds)) for ds in range(4)] for tt in range(NT)]
                for tt in range(NT):
                    P.op("pool", lambda e, tt=tt: e.memset(acc[:, tt, :], 0.0), w=r_acc[tt])
                phm = ExitStack()

                def sbm(name, shape, dt):
                    return phm.enter_context(nc.sbuf_tensor(name, shape, dt))

                if half == 0:
                    hT = sbm("h2Tr", [128, KC, T], BF16)
                    r_hT = P.res("h2Tr")
                    P.dma("sp", hT[:, 0:16, :], h2T_d[:, 0:16, :], w=[r_hT])
                    P.dma("sp", hT[:, 16:32, :], h2T_d[:, 16:32, :], w=[r_hT])
                    ub = Buf(P, sbm, "ub", [128, KC, 256], BF16, 2)
                    gtb = Buf(P, sbm, "gtb", [128, 2, T], BF16, 2)
                    aps = Buf(P, psp, "aps", [128, 512], F32, 4)
                GB = 2 if half == 0 else 8
                NG = 128 // GB
                vb_ = Buf(P, sbm, "vb%d" % half, [128, GB, 2048], BF16, 2)
                ab = Buf(P, sbm, "ab%d" % half, [128, GB, T], BF16, 2)
                ops_ = Buf(P, psp, "ops%d" % half, [128, 512], F32, 4)
                uT_v = uT.rearrange("(kc p) e -> p kc e", p=128)
                d0 = half * 2048

                def prefetch(p):
                    res = {}
                    if half == 0:
                        ut, ur = ub.next()
                        P.dma("pool", ut[:], uT_v[:, :, p * 256:(p + 1) * 256], w=[ur])
                        gt, gr = gtb.next()
                        P.dma("sp", gt[:], GT_d[2 * p:2 * p + 2].rearrange("i j t -> j i t"), w=[gr])
                        res["u"] = (ut, ur)
                        res["g"] = (gt, gr)
                    vt, vr = vb_.next()
                    for b2 in range(0, GB, 2):
                        P.dma("pool", vt[:, b2:b2 + 2, :],
                              vtab[(p * GB + b2) * 128:(p * GB + b2 + 2) * 128, d0:d0 + 2048].rearrange("(b e) d -> e b d", b=2),
                              w=[vr])
                    res["v"] = (vt, vr)
                    at, ar = ab.next()
                    if half == 1:
                        for b2 in range(0, GB, 2):
                            P.dma("sp", at[:, b2:b2 + 2, :], actg_d[p * GB + b2:p * GB + b2 + 2].rearrange("b e t -> e b t"), w=[ar])
                    res["a"] = (at, ar)
                    return res

                nxt = prefetch(0)
                for p in range(NG):
                    cur = nxt
                    if p + 1 < NG:
                        nxt = prefetch(p + 1)
                    at, ar = cur["a"]
                    vt, vr = cur["v"]
                    if half == 0:
                        ut, ur = cur["u"]
                        gt, gr = cur["g"]
                        for b in range(2):
                            for hf in range(2):
                                pt, pr = aps.next()
                                for kc in range(KC):
                                    P.op("pe", lambda e, pt=pt, ut=ut, kc=kc, b=b, hf=hf: e.matmul(
                                        pt[:], lhsT=ut[:, kc, b * 128:(b + 1) * 128], rhs=hT[:, kc, hf * 512:(hf + 1) * 512],
                                        start=(kc == 0), stop=(kc == KC - 1)), r=[ur, r_hT], w=[pr], inc=(kc == KC - 1))
                                P.op("act", lambda e, pt=pt, at=at, b=b, hf=hf: e.activation(
                                    out=at[:, b, hf * 512:(hf + 1) * 512], in_=pt[:], func=AF.Gelu), r=[pr], w=[ar])
                        P.op("dve", lambda e, at=at, gt=gt: e.tensor_tensor(
                            out=at[:].rearrange("p b t -> p (b t)"), in0=at[:].rearrange("p b t -> p (b t)"),
                            in1=gt[:].rearrange("p b t -> p (b t)"), op=ALU.mult), r=[ar, gr], w=[ar])
                        P.dma("sp", actg_d[2 * p:2 * p + 2].rearrange("b e t -> e b t"), at[:], r=[ar])
                    for tt in range(NT):
                        for ds in range(4):
                            ot, orr = ops_.next()
                            for b in range(GB):
                                P.op("pe", lambda e, ot=ot, at=at, vt=vt, b=b, tt=tt, ds=ds: e.matmul(
                                    ot[:], lhsT=at[:, b, tt * 128:(tt + 1) * 128], rhs=vt[:, b, ds * 512:(ds + 1) * 512],
                                    start=(b == 0), stop=(b == GB - 1)), r=[ar, vr], w=[orr], inc=(b == GB - 1))
                            dst = acc[:, tt, ds * 512:(ds + 1) * 512]
                            P.op("dve", lambda e, ot=ot, dst=dst: e.tensor_tensor(out=dst, in0=ot[:], in1=dst, op=ALU.add),
                                 r=[orr, r_acc[tt][ds]], w=[r_acc[tt][ds]])
                P.barrier()
                phm.close()
                g2 = sbp("g2bc%d" % half, [128, 2048], F32)
                r_g2 = P.res("g2bc")
                P.dma("sp", g2[:], mod_d[5 * D + d0:5 * D + d0 + 2048].partition_broadcast(128), w=[r_g2])
                xr_ = Buf(P, sbp, "x1r%d" % half, [128, 2048], F32, 2)
                for tt in range(NT):
                    xt, xr = xr_.next()
                    P.dma("sp", xt[:], x1_d[tt * 128:(tt + 1) * 128, d0:d0 + 2048], w=[xr])
                    P.op("dve", lambda e, tt=tt: e.tensor_tensor(out=acc[:, tt, :], in0=acc[:, tt, :], in1=g2[:], op=ALU.mult),
                         r=r_acc[tt] + [r_g2], w=r_acc[tt])
                    P.op("pool", lambda e, tt=tt, xt=xt: e.tensor_tensor(out=acc[:, tt, :], in0=acc[:, tt, :], in1=xt[:], op=ALU.add),
                         r=r_acc[tt] + [xr], w=r_acc[tt])
                    P.op("act", lambda e, tt=tt, xt=xt, half=half: e.activation(
                        out=xt[:], in_=acc[:, tt, :], func=AF.Square, accum_out=ssq[:, half, tt:tt + 1]),
                        r=r_acc[tt], w=[xr, r_ssq])
                    P.dma("sp", y_d[tt * 128:(tt + 1) * 128, d0:d0 + 2048], acc[:, tt, :], r=r_acc[tt])
                P.barrier()
        with ExitStack() as ph:
            def sbp(name, shape, dt):
                return ph.enter_context(nc.sbuf_tensor(name, shape, dt))

            lnf = sbp("lnfbc", [128, D], F32)
            r_lnf = P.res("lnfbc")
            P.dma("sp", lnf[:], lnf_row.partition_broadcast(128), w=[r_lnf])
            rs = sbp("rsf", [128, NT], F32)
            P.op("dve", lambda e: e.tensor_tensor(out=rs[:], in0=ssq[:, 0, :], in1=ssq[:, 1, :], op=ALU.add), r=[r_ssq], w=[r_ssq])
            P.op("act", lambda e: e.activation(out=rs[:], in_=rs[:], func=AF.Sqrt, scale=1.0 / D, bias=EPS), r=[r_ssq], w=[r_ssq])
            P.op("dve", lambda e: e.reciprocal(out=rs[:], in_=rs[:]), r=[r_ssq], w=[r_ssq])
            yb = Buf(P, sbp, "yfin", [128, D], F32, 2)
            for tt in range(NT):
                yt, yr = yb.next()
                P.dma("sp", yt[:], y_d[tt * 128:(tt + 1) * 128, :], w=[yr])
                P.op("dve", lambda e, yt=yt, tt=tt: e.scalar_tensor_tensor(
                    out=yt[:], in0=yt[:], scalar=rs[:, tt:tt + 1], in1=lnf[:], op0=ALU.mult, op1=ALU.mult),
                    r=[yr, r_ssq, r_lnf], w=[yr])
                P.dma("sp", out_d[tt * 128:(tt + 1) * 128, :], yt[:], r=[yr])
            P.barrier()


_NC_CACHE = {}


def kernel(**inputs):
    stage = 4
    if "nc" not in _NC_CACHE:
        _NC_CACHE["nc"] = build(stage=stage)
    nc = _NC_CACHE["nc"]
    shared = prep_shared(inputs, stage)
    in_maps = []
    for core in range(8):
        b, q = core // 4, core % 4
        in_maps.append(prep_core(inputs, shared, b, q))
    res = run_bass_kernel_spmd(nc, in_maps, core_ids=list(range(8)))
    out = np.zeros((2, 4096, D), dtype=np.float32)
    for core in range(8):
        b, q = core // 4, core % 4
        out[b, q::4, :] = np.asarray(res.results[core]["out"], dtype=np.float32)
    return out
```
